# Optimizing a Trainium2 kernel written in Bass

```python
import math
import jax, jax.numpy as jnp
from jax import lax
import numpy as np

D_MODEL = 2048
BATCH = 8
SEQ = 2048
DEPTH = 4

MOBA_HEADS = 16
MOBA_HEAD_DIM = 64
MOBA_DIM = MOBA_HEADS * MOBA_HEAD_DIM
MOBA_BLOCK = 256
MOBA_TOPK = 3
MOBA_Q_CHUNK = 16
RWKV_HEADS = 16
RWKV_HEAD_DIM = 64
RWKV_DIM = RWKV_HEADS * RWKV_HEAD_DIM
RWKV_DECAY_LORA = 64
RWKV_A_LORA = 64
RWKV_V_LORA = 32
RWKV_G_LORA = 160
RWKV_GN_EPS = 64e-5
RWKV_IN = 3 * RWKV_DIM + RWKV_DECAY_LORA + RWKV_A_LORA + RWKV_G_LORA
EVEN_IN = 3 * MOBA_DIM + RWKV_IN
EVEN_MIX = MOBA_DIM + RWKV_DIM
MLA_HEADS = 16
MLA_Q_LORA = 512
MLA_KV_LORA = 512
MLA_NOPE_DIM = 128
MLA_ROPE_DIM = 64
MLA_V_DIM = 128
MLA_Q_BLOCK = 128
ROPE_THETA = 10000.0
ODD_IN = MLA_Q_LORA + MLA_KV_LORA + MLA_ROPE_DIM
ODD_MIX = MLA_HEADS * MLA_V_DIM
N_EXPERTS = 32
TOP_K = 4
D_EXPERT = 1024
SWIGLU_ALPHA = 1.702
SWIGLU_LIMIT = 7.0
MOE_ROW_BLOCK = 128
N_EVEN = (DEPTH + 1) // 2
N_ODD = DEPTH // 2
DEEPNORM_ALPHA = (2 * DEPTH) ** 0.25
DEEPNORM_BETA = (8 * DEPTH) ** -0.25
LN_EPS = 1e-5
RMS_EPS = 1e-6
NEG_INF = -1e30

kernel_name = "hybrid_moba_rwkv7_mla_moe_deepnorm"


def layer_norm(x, g, b):
    xf = x.astype(jnp.float32)
    mu = jnp.mean(xf, axis=-1, keepdims=True)
    var = jnp.mean(jnp.square(xf - mu), axis=-1, keepdims=True)
    return ((xf - mu) * lax.rsqrt(var + LN_EPS) * g.astype(jnp.float32) + b.astype(jnp.float32)).astype(x.dtype)


def rms_norm(x, g):
    xf = x.astype(jnp.float32)
    ms = jnp.mean(jnp.square(xf), axis=-1, keepdims=True)
    return (xf * lax.rsqrt(ms + RMS_EPS) * g.astype(jnp.float32)).astype(x.dtype)


def token_shift(z):
    return jnp.pad(z, ((0, 0), (1, 0), (0, 0)))[:, :-1]


def rope_tables(S, dim, dtype):
    inv = ROPE_THETA ** (-jnp.arange(0, dim, 2, dtype=jnp.float32) / dim)
    ang = jnp.arange(S, dtype=jnp.float32)[:, None] * inv[None, :]
    return jnp.cos(ang).astype(dtype), jnp.sin(ang).astype(dtype)


def apply_rope(x, cos, sin):
    x1, x2 = jnp.split(x, 2, axis=-1)
    return jnp.concatenate([x1 * cos - x2 * sin, x1 * sin + x2 * cos], axis=-1)


def moba_attention(q, k, v):
    B, H, S, Dh = q.shape
    nb = -(-S // MOBA_BLOCK)
    pad = nb * MOBA_BLOCK - S
    kb = jnp.pad(k, ((0, 0), (0, 0), (0, pad), (0, 0))).reshape(B, H, nb, MOBA_BLOCK, Dh)
    vb = jnp.pad(v, ((0, 0), (0, 0), (0, pad), (0, 0))).reshape(B, H, nb, MOBA_BLOCK, Dh)
    k_mean = jnp.mean(kb.astype(jnp.float32), axis=3)
    n_sel = min(MOBA_TOPK, nb)
    slopes = 2.0 ** (-8.0 * jnp.arange(1, H + 1, dtype=jnp.float32) / H)
    scale = Dh ** -0.5
    b_ix = jnp.arange(B)[:, None, None, None]
    h_ix = jnp.arange(H)[None, :, None, None]
    blk_pos = jnp.arange(MOBA_BLOCK)
    qc_len = MOBA_Q_CHUNK

    def chunk(c):
        t0 = c * qc_len
        qc = lax.dynamic_slice_in_dim(q, t0, qc_len, axis=2)
        t = t0 + jnp.arange(qc_len)
        cur = t0 // MOBA_BLOCK
        gate = jnp.einsum('bhqd,bhnd->bhqn', qc.astype(jnp.float32), k_mean)
        gate = jnp.where(jnp.arange(nb) < cur, gate, NEG_INF)
        _, idx = lax.top_k(gate, n_sel)
        valid = idx < cur
        k_sel = kb[b_ix, h_ix, idx]
        v_sel = vb[b_ix, h_ix, idx]
        dist_sel = (t[:, None, None] - (idx[..., None] * MOBA_BLOCK + blk_pos)).astype(jnp.float32)
        s_sel = (jnp.einsum('bhqd,bhqrkd->bhqrk', qc, k_sel).astype(jnp.float32) * scale
                 - slopes[:, None, None, None] * dist_sel)
        s_sel = jnp.where(valid[..., None], s_sel, NEG_INF)
        k_cur = lax.dynamic_index_in_dim(kb, cur, axis=2, keepdims=False)
        v_cur = lax.dynamic_index_in_dim(vb, cur, axis=2, keepdims=False)
        dist_cur = t[:, None] - (cur * MOBA_BLOCK + blk_pos)[None, :]
        s_cur = (jnp.einsum('bhqd,bhkd->bhqk', qc, k_cur).astype(jnp.float32) * scale
                 - slopes[:, None, None] * dist_cur.astype(jnp.float32))
        s_cur = jnp.where(dist_cur >= 0, s_cur, NEG_INF)
        s = jnp.concatenate([s_sel.reshape(B, H, qc_len, n_sel * MOBA_BLOCK), s_cur], axis=-1)
        p = jax.nn.softmax(s, axis=-1).astype(v.dtype)
        p_sel = p[..., :n_sel * MOBA_BLOCK].reshape(B, H, qc_len, n_sel, MOBA_BLOCK)
        p_cur = p[..., n_sel * MOBA_BLOCK:]
        return (jnp.einsum('bhqrk,bhqrkd->bhqd', p_sel, v_sel)
                + jnp.einsum('bhqk,bhkd->bhqd', p_cur, v_cur))

    out = lax.map(chunk, jnp.arange(S // qc_len))
    return out.transpose(1, 0, 3, 2, 4).reshape(B, S, H * Dh)


def rwkv7_scan(r, decay, k, v, a, b):
    B, S, H, N = r.shape

    def step(state, inp):
        r_t, w_t, k_t, v_t, a_t, b_t = inp
        sa = jnp.einsum('bhij,bhj->bhi', state, a_t)
        state = (state * w_t[:, :, None, :] + sa[..., None] * b_t[:, :, None, :]
                 + v_t[..., None] * k_t[:, :, None, :])
        return state, jnp.einsum('bhij,bhj->bhi', state, r_t)

    xs = tuple(jnp.moveaxis(z.astype(jnp.float32), 1, 0) for z in (r, decay, k, v, a, b))
    state0 = jnp.zeros((B, H, N, N), jnp.float32)
    _, y = lax.scan(step, state0, xs)
    return jnp.moveaxis(y, 0, 1)


def rwkv7_mix(p, mu, w0, w2, a0, a2, g2, k_k, k_a, r_k, ln_w, ln_b, v_first, v_lora):
    B, S, _ = p.shape
    C, H, N = RWKV_DIM, RWKV_HEADS, RWKV_HEAD_DIM
    p = p + (token_shift(p) - p) * mu
    r, k, v = p[..., :C], p[..., C:2 * C], p[..., 2 * C:3 * C]
    o = 3 * C
    wd = p[..., o:o + RWKV_DECAY_LORA]
    ad = p[..., o + RWKV_DECAY_LORA:o + RWKV_DECAY_LORA + RWKV_A_LORA]
    gd = p[..., o + RWKV_DECAY_LORA + RWKV_A_LORA:]
    w = -jax.nn.softplus(-(w0 + jnp.tanh(wd) @ w2)) - 0.5
    decay = jnp.exp(-jnp.exp(w.astype(jnp.float32)))
    a = jax.nn.sigmoid(a0 + ad @ a2)
    g = jax.nn.sigmoid(gd) @ g2
    if v_lora is not None:
        v0, v1, v2 = v_lora
        v = v + (v_first - v) * jax.nn.sigmoid(v0 + (v @ v1) @ v2)
    heads = lambda z: z.reshape(B, S, H, N)
    kk = heads(k * k_k).astype(jnp.float32)
    kk = kk * lax.rsqrt(jnp.maximum(jnp.sum(jnp.square(kk), axis=-1, keepdims=True), 1e-24))
    k = k * (1.0 + (a - 1.0) * k_a)
    a_h = heads(a).astype(jnp.float32)
    y = rwkv7_scan(heads(r), heads(decay), heads(k), heads(v), -kk, kk * a_h)
    y_mu = jnp.mean(y, axis=-1, keepdims=True)
    y_var = jnp.mean(jnp.square(y - y_mu), axis=-1, keepdims=True)
    y = ((y - y_mu) * lax.rsqrt(y_var + RWKV_GN_EPS) * ln_w.reshape(H, N).astype(jnp.float32)
         + ln_b.reshape(H, N).astype(jnp.float32))
    bonus = jnp.sum((heads(r) * heads(k) * r_k).astype(jnp.float32), axis=-1, keepdims=True)
    y = y + bonus * heads(v).astype(jnp.float32)
    out = (y.reshape(B, S, C) * g.astype(jnp.float32)).astype(p.dtype)
    return out, v


def mla_attention(h, w_in, q_norm, kv_norm, w_uq, w_ukv):
    B, S, _ = h.shape
    H = MLA_HEADS
    proj = h @ w_in
    c_q = proj[..., :MLA_Q_LORA]
    c_kv = proj[..., MLA_Q_LORA:MLA_Q_LORA + MLA_KV_LORA]
    k_rope = proj[..., MLA_Q_LORA + MLA_KV_LORA:]
    q = (rms_norm(c_q, q_norm) @ w_uq).reshape(B, S, H, MLA_NOPE_DIM + MLA_ROPE_DIM)
    kv = (rms_norm(c_kv, kv_norm) @ w_ukv).reshape(B, S, H, MLA_NOPE_DIM + MLA_V_DIM)
    q_nope, q_rope = q[..., :MLA_NOPE_DIM], q[..., MLA_NOPE_DIM:]
    k_nope, v = kv[..., :MLA_NOPE_DIM], kv[..., MLA_NOPE_DIM:]
    cos, sin = rope_tables(S, MLA_ROPE_DIM, h.dtype)
    q_rope = apply_rope(q_rope, cos[:, None, :], sin[:, None, :])
    k_rope = apply_rope(k_rope, cos, sin)
    scale = (MLA_NOPE_DIM + MLA_ROPE_DIM) ** -0.5
    k_pos = jnp.arange(S)

    def block(i):
        t0 = i * MLA_Q_BLOCK
        qn = lax.dynamic_slice_in_dim(q_nope, t0, MLA_Q_BLOCK, axis=1)
        qr = lax.dynamic_slice_in_dim(q_rope, t0, MLA_Q_BLOCK, axis=1)
        s = (jnp.einsum('bqhd,bkhd->bhqk', qn, k_nope)
             + jnp.einsum('bqhd,bkd->bhqk', qr, k_rope)).astype(jnp.float32) * scale
        q_pos = t0 + jnp.arange(MLA_Q_BLOCK)
        s = jnp.where(k_pos[None, :] <= q_pos[:, None], s, NEG_INF)
        p = jax.nn.softmax(s, axis=-1).astype(v.dtype)
        return jnp.einsum('bhqk,bkhd->bqhd', p, v)

    o = lax.map(block, jnp.arange(S // MLA_Q_BLOCK))
    return o.transpose(1, 0, 2, 3, 4).reshape(B, S, H * MLA_V_DIM)


def moe_ffn(h, w_r, b_r, w1, b1, w2, b2):
    B, S, D = h.shape
    x = h.reshape(-1, D)
    T = x.shape[0]
    R = MOE_ROW_BLOCK
    logits = x.astype(jnp.float32) @ w_r.astype(jnp.float32) + b_r.astype(jnp.float32)
    top_val, top_idx = lax.top_k(logits, TOP_K)
    gates = jax.nn.softmax(top_val, axis=-1)
    M = T * TOP_K
    flat_e = top_idx.reshape(M)
    flat_tok = jnp.arange(M) // TOP_K
    flat_g = gates.reshape(M)
    order = jnp.argsort(flat_e)
    e_sorted, tok_sorted, g_sorted = flat_e[order], flat_tok[order], flat_g[order]
    counts = jnp.bincount(flat_e, length=N_EXPERTS)
    padded = (counts + R - 1) // R * R
    pad_end = jnp.cumsum(padded)
    pad_start = pad_end - padded
    start = jnp.cumsum(counts) - counts
    dest = pad_start[e_sorted] + (jnp.arange(M) - start[e_sorted])
    n_blocks = (M + N_EXPERTS * (R - 1) + R - 1) // R
    rows = n_blocks * R
    xbuf = jnp.zeros((rows, D), x.dtype).at[dest].set(x[tok_sorted])
    block_e = jnp.minimum(jnp.searchsorted(pad_end, jnp.arange(n_blocks) * R, side='right'), N_EXPERTS - 1)

    def expert_block(args):
        xb, e = args
        hgu = xb @ w1[e] + b1[e]
        gate = jnp.minimum(hgu[:, :D_EXPERT], SWIGLU_LIMIT)
        up = jnp.clip(hgu[:, D_EXPERT:], -SWIGLU_LIMIT, SWIGLU_LIMIT)
        act = gate * jax.nn.sigmoid(SWIGLU_ALPHA * gate) * (up + 1.0)
        return act @ w2[e] + b2[e]

    ybuf = lax.map(expert_block, (xbuf.reshape(n_blocks, R, D), block_e)).reshape(rows, D)
    y = ybuf[dest] * g_sorted[:, None].astype(x.dtype)
    out = jnp.zeros((T, D), x.dtype).at[tok_sorted].add(y)
    return out.reshape(B, S, D)


def setup_inputs(seed: int = 0) -> dict:
    key = jax.random.key(seed)
    ks = iter(jax.random.split(key, 40))

    def nrm(shape, scale):
        return jax.random.normal(next(ks), shape, jnp.float32) * scale

    def unif(shape, lo, hi):
        return jax.random.uniform(next(ks), shape, jnp.float32, lo, hi)

    D, C = D_MODEL, RWKV_DIM
    NV = max(N_EVEN - 1, 0)
    return {
        'x': nrm((BATCH, SEQ, D), 1.0),
        'ev_w_in': nrm((N_EVEN, D, EVEN_IN), D ** -0.5),
        'ev_w_out': nrm((N_EVEN, EVEN_MIX, D), EVEN_MIX ** -0.5 * DEEPNORM_BETA),
        'rw_mu': unif((N_EVEN, RWKV_IN), 0.0, 1.0),
        'rw_w0': unif((N_EVEN, C), -5.0, -0.5),
        'rw_w2': nrm((N_EVEN, RWKV_DECAY_LORA, C), 0.1 * RWKV_DECAY_LORA ** -0.5),
        'rw_a0': nrm((N_EVEN, C), 0.1),
        'rw_a2': nrm((N_EVEN, RWKV_A_LORA, C), 0.1 * RWKV_A_LORA ** -0.5),
        'rw_g2': nrm((N_EVEN, RWKV_G_LORA, C), RWKV_G_LORA ** -0.5),
        'rw_k_k': 0.85 + nrm((N_EVEN, C), 0.05),
        'rw_k_a': 1.0 + nrm((N_EVEN, C), 0.05),
        'rw_r_k': nrm((N_EVEN, RWKV_HEADS, RWKV_HEAD_DIM), 0.1),
        'rw_ln_w': 1.0 + nrm((N_EVEN, C), 0.05),
        'rw_ln_b': nrm((N_EVEN, C), 0.01),
        'rw_v0': 1.0 + nrm((NV, C), 0.1),
        'rw_v1': nrm((NV, C, RWKV_V_LORA), C ** -0.5),
        'rw_v2': nrm((NV, RWKV_V_LORA, C), 0.1 * RWKV_V_LORA ** -0.5),
        'od_w_in': nrm((N_ODD, D, ODD_IN), D ** -0.5),
        'od_q_norm': 1.0 + nrm((N_ODD, MLA_Q_LORA), 0.05),
        'od_kv_norm': 1.0 + nrm((N_ODD, MLA_KV_LORA), 0.05),
        'od_w_uq': nrm((N_ODD, MLA_Q_LORA, MLA_HEADS * (MLA_NOPE_DIM + MLA_ROPE_DIM)), MLA_Q_LORA ** -0.5),
        'od_w_ukv': nrm((N_ODD, MLA_KV_LORA, MLA_HEADS * (MLA_NOPE_DIM + MLA_V_DIM)), MLA_KV_LORA ** -0.5),
        'od_w_out': nrm((N_ODD, ODD_MIX, D), ODD_MIX ** -0.5 * DEEPNORM_BETA),
        'ln_mix_g': 1.0 + nrm((DEPTH, D), 0.05),
        'ln_mix_b': nrm((DEPTH, D), 0.01),
        'ln_ffn_g': 1.0 + nrm((DEPTH, D), 0.05),
        'ln_ffn_b': nrm((DEPTH, D), 0.01),
        'moe_w_r': nrm((DEPTH, D, N_EXPERTS), D ** -0.5),
        'moe_b_r': nrm((DEPTH, N_EXPERTS), 0.01),
        'moe_w1': nrm((DEPTH, N_EXPERTS, D, 2 * D_EXPERT), D ** -0.5),
        'moe_b1': nrm((DEPTH, N_EXPERTS, 2 * D_EXPERT), 0.01),
        'moe_w2': nrm((DEPTH, N_EXPERTS, D_EXPERT, D), D_EXPERT ** -0.5 * DEEPNORM_BETA),
        'moe_b2': nrm((DEPTH, N_EXPERTS, D), 0.01),
    }


def reference(x, ev_w_in, ev_w_out, rw_mu, rw_w0, rw_w2, rw_a0, rw_a2, rw_g2, rw_k_k, rw_k_a,
              rw_r_k, rw_ln_w, rw_ln_b, rw_v0, rw_v1, rw_v2, od_w_in, od_q_norm, od_kv_norm,
              od_w_uq, od_w_ukv, od_w_out, ln_mix_g, ln_mix_b, ln_ffn_g, ln_ffn_b,
              moe_w_r, moe_b_r, moe_w1, moe_b1, moe_w2, moe_b2):
    B, S, _ = x.shape
    h = x
    v_first = None
    for layer in range(DEPTH):
        j = layer // 2
        if layer % 2 == 0:
            proj = h @ ev_w_in[j]
            to_heads = lambda z: z.reshape(B, S, MOBA_HEADS, MOBA_HEAD_DIM).transpose(0, 2, 1, 3)
            q = to_heads(proj[..., :MOBA_DIM])
            k = to_heads(proj[..., MOBA_DIM:2 * MOBA_DIM])
            v = to_heads(proj[..., 2 * MOBA_DIM:3 * MOBA_DIM])
            a_out = moba_attention(q, k, v)
            v_lora = None if j == 0 else (rw_v0[j - 1], rw_v1[j - 1], rw_v2[j - 1])
            b_out, rw_val = rwkv7_mix(proj[..., 3 * MOBA_DIM:], rw_mu[j], rw_w0[j], rw_w2[j],
                                      rw_a0[j], rw_a2[j], rw_g2[j], rw_k_k[j], rw_k_a[j],
                                      rw_r_k[j], rw_ln_w[j], rw_ln_b[j], v_first, v_lora)
            if j == 0:
                v_first = rw_val
            mix = jnp.concatenate([a_out, b_out], axis=-1) @ ev_w_out[j]
        else:
            mix = mla_attention(h, od_w_in[j], od_q_norm[j], od_kv_norm[j],
                                od_w_uq[j], od_w_ukv[j]) @ od_w_out[j]
        h = layer_norm(DEEPNORM_ALPHA * h + mix, ln_mix_g[layer], ln_mix_b[layer])
        ffn = moe_ffn(h, moe_w_r[layer], moe_b_r[layer], moe_w1[layer], moe_b1[layer],
                      moe_w2[layer], moe_b2[layer])
        h = layer_norm(DEEPNORM_ALPHA * h + ffn, ln_ffn_g[layer], ln_ffn_b[layer])
    return h
```

```python
import numpy as np
import concourse.bass as bass
import concourse.mybir as mybir
from concourse.bass_utils import run_bass_kernel_spmd

F32 = mybir.dt.float32
BF16 = mybir.dt.bfloat16
I32 = mybir.dt.int32
U32 = mybir.dt.uint32
ALU = mybir.AluOpType
AF = mybir.ActivationFunctionType
AX = mybir.AxisListType

ENGS = ("pe", "act", "dve", "pool", "sp")
NSLOT = 48

S = 2048
D = 2048
NT = 16
DEPTH = 4
ALPHA = (2 * DEPTH) ** 0.25
NE = 32
CAP = 384
DEXP = 1024


class Buf:
    __slots__ = ("w", "r")

    def __init__(self):
        self.w = None
        self.r = []


class Tl:
    __slots__ = ("t", "b")

    def __init__(self, t):
        self.t = t
        self.b = Buf()

    def __getitem__(self, k):
        return self.t[k]


def _bufs(xs):
    out = []
    for x in xs:
        out.append(x.b if isinstance(x, Tl) else x)
    return out


class _Rec:
    def __init__(self):
        self.call = None

    def __getattr__(self, name):
        def f(*a, **kw):
            self.call = (name, a, kw)
            return self
        return f


BCREG = "__bcreg__"


def _eager(fn):
    r = _Rec()
    fn(r)
    name, a, kw = r.call

    def replay(eo, prog):
        kw2 = {k: (prog.bc_reg if (isinstance(v, str) and v == BCREG) else v) for k, v in kw.items()}
        return getattr(eo, name)(*a, **kw2)
    return replay


class Prog:
    def __init__(self, nc):
        self.nc = nc
        self.ops = []
        self.sb_off = 16640
        self.sb_mark = 16640
        self.uid = 0

    def sb(self, shape, dtype, name=None):
        self.uid += 1
        nm = f"{name or 't'}_{self.uid}"
        esz = {F32: 4, BF16: 2, I32: 4, U32: 4}[dtype]
        n = int(np.prod(shape[1:])) * esz
        off = (self.sb_off + 63) // 64 * 64
        t = self.nc.alloc_sbuf_tensor_at(nm, list(shape), dtype, offset=off)
        self.sb_off = off + n
        assert self.sb_off <= 229376, f"SBUF overflow {self.sb_off}"
        return Tl(t)

    def sb_at(self, off, shape, dtype, name):
        self.uid += 1
        return Tl(self.nc.alloc_sbuf_tensor_at(f"{name}_{self.uid}", list(shape), dtype, offset=off))

    def mark(self):
        self.sb_mark = self.sb_off

    def reset(self):
        self.sb_off = self.sb_mark

    def op(self, eng, fn, reads=(), writes=()):
        self.ops.append(("c", eng, _eager(fn), tuple(_bufs(reads)), tuple(_bufs(writes))))

    def dma(self, eng, fn, reads=(), writes=()):
        self.ops.append(("d", eng, _eager(fn), tuple(_bufs(reads)), tuple(_bufs(writes))))

    def barrier(self):
        self.ops.append(("b", None, None, (), ()))

    def emit(self):
        nc = self.nc
        esem = {e: nc.alloc_semaphore(name=f"s_{e}") for e in ENGS}
        dsem = [nc.alloc_semaphore(name=f"d_{i}") for i in range(NSLOT)]
        cnt = {e: 0 for e in ENGS}
        dtarget = [0] * NSLOT
        dnext = 0
        dnext_sw = 0
        seen = {e: {} for e in ENGS}
        streams = {e: [] for e in ENGS}

        def need(e, key, waits):
            if key is None:
                return
            kind, a, v = key
            if kind == "e" and a == "pe" and e == "pe":
                return
            sk = (kind, a)
            if seen[e].get(sk, 0) >= v:
                return
            seen[e][sk] = v
            waits.append((esem[a] if kind == "e" else dsem[a], v))

        for (kind, eng, fn, reads, writes) in self.ops:
            if kind == "b":
                for e in ENGS:
                    waits = []
                    for e2 in ENGS:
                        if cnt[e2] > 0:
                            need(e, ("e", e2, cnt[e2]), waits)
                    for s in range(NSLOT):
                        if dtarget[s] > 0:
                            need(e, ("d", s, dtarget[s]), waits)
                    if waits:
                        streams[e].append((waits, None, None))
                continue
            waits = []
            for b in reads:
                need(eng, b.w, waits)
            for b in writes:
                need(eng, b.w, waits)
                for k in b.r:
                    need(eng, k, waits)
            if kind == "c":
                cnt[eng] += 1
                key = ("e", eng, cnt[eng])
                streams[eng].append((waits, fn, (esem[eng], 1)))
            else:
                half = NSLOT // 2
                if eng == "pool":
                    s = half + dnext_sw
                    dnext_sw = (dnext_sw + 1) % half
                else:
                    s = dnext
                    dnext = (dnext + 1) % half
                need(eng, ("d", s, dtarget[s]) if dtarget[s] else None, waits)
                dtarget[s] += 16
                key = ("d", s, dtarget[s])
                streams[eng].append((waits, fn, (dsem[s], 16)))
            for b in reads:
                b.r.append(key)
            for b in writes:
                b.w = key
                b.r = []
        final_waits = []
        for s in range(NSLOT):
            if dtarget[s]:
                final_waits.append((dsem[s], dtarget[s]))
        for e2 in ENGS:
            if cnt[e2]:
                final_waits.append((esem[e2], cnt[e2]))
        streams["sp"].append((final_waits, None, None))
        self.stats = {e: len(streams[e]) for e in ENGS}
        engobj = {"pe": nc.tensor, "act": nc.scalar, "dve": nc.vector,
                  "pool": nc.gpsimd, "sp": nc.sync}

        def run(e):
            eo = engobj[e]
            if e == "pool":
                self.bc_reg = eo.to_reg(NE * CAP - 1)
            for (waits, fn, inc) in streams[e]:
                for (sem, v) in waits:
                    eo.wait_ge(sem, v)
                if fn is not None:
                    fn(eo, self).then_inc(inc[0], inc[1])

        with nc.Block() as block:
            @block.tensor
            def _(t):
                run("pe")

            @block.scalar
            def _(t):
                run("act")

            @block.vector
            def _(t):
                run("dve")

            @block.gpsimd
            def _(t):
                run("pool")

            @block.sync
            def _(t):
                run("sp")


class Ctx:
    pass


def setup_common(nc, P, C):
    C.nc, C.P = nc, P
    C.psF = [Tl(nc.alloc_psum_tensor(f"psF{i}", [128, 512], F32)) for i in range(6)]
    C.psB = [Tl(nc.alloc_psum_tensor(f"psB{i}", [128, 1024], BF16)) for i in range(2)]
    C.psF_i = 0
    C.psB_i = 0
    C.identB = P.sb([128, 128], BF16, "identB")
    C.identF = P.sb([128, 128], F32, "identF")
    C.onesB = P.sb([128, 128], BF16, "onesB")
    C.triB = P.sb([128, 128], BF16, "triB")
    C.iotaC = P.sb([128, CAP], F32, "iotaC")
    C.eoff = P.sb([128, NE], F32, "eoff")
    C.epsln = P.sb([128, 1], F32, "epsln")
    P.op("pool", lambda e: e.memset(C.epsln[:], 1e-5), writes=[C.epsln])
    for t, val in ((C.identB, 1.0), (C.identF, 1.0), (C.onesB, 1.0), (C.triB, 1.0)):
        P.op("pool", lambda e, t=t, val=val: e.memset(t[:], val), writes=[t])
    for t in (C.identB, C.identF):
        P.op("pool", lambda e, t=t: e.affine_select(out=t[:], in_=t[:], pattern=[[-1, 128]],
             compare_op=ALU.is_equal, fill=0.0, base=0, channel_multiplier=1), reads=[t], writes=[t])
    P.op("pool", lambda e: e.affine_select(out=C.triB[:], in_=C.triB[:], pattern=[[1, 128]],
         compare_op=ALU.is_gt, fill=0.0, base=0, channel_multiplier=-1), reads=[C.triB], writes=[C.triB])
    P.op("pool", lambda e: e.iota(C.iotaC[:], pattern=[[1, CAP]], base=0, channel_multiplier=0,
         allow_small_or_imprecise_dtypes=True), writes=[C.iotaC])
    P.op("pool", lambda e: e.iota(C.eoff[:], pattern=[[CAP, NE]], base=0, channel_multiplier=0,
         allow_small_or_imprecise_dtypes=True), writes=[C.eoff])


def psf(C, pool=None):
    if pool is None:
        t = C.psF[C.psF_i % len(C.psF)]
        C.psF_i += 1
        return t
    if not hasattr(C, "ps_pool_i"):
        C.ps_pool_i = {}
    i = C.ps_pool_i.get(pool, 0)
    C.ps_pool_i[pool] = i + 1
    return C.psF[pool[i % len(pool)]]


def psb(C):
    t = C.psB[C.psB_i % len(C.psB)]
    C.psB_i += 1
    return t


def bcast_rows(ap_row, n=128):
    return ap_row.broadcast_to([n, ap_row.shape[-1]])


def stage_ln(C, *, h_in, add_mode, h_out, g_row, b_row, hT=None, xb_out=None, moe=None,
             mix_hbm=None, out_final=None):
    nc, P = C.nc, C.P
    P.barrier()
    P.reset()
    gB = P.sb([128, D], F32, "gB")
    bB = P.sb([128, D], F32, "bB")
    P.dma("sp", lambda e: e.dma_start(out=gB[:], in_=bcast_rows(g_row)), writes=[gB])
    P.dma("sp", lambda e: e.dma_start(out=bB[:], in_=bcast_rows(b_row)), writes=[bB])
    nb = 2
    hin = [P.sb([128, D], F32, "hin") for _ in range(nb)]
    add = [P.sb([128, D], F32, "add") for _ in range(nb)]
    ytl = [P.sb([128, D], F32, "ytl") for _ in range(4)] if add_mode == "moe" else None
    xbt = [P.sb([128, D], BF16, "xbt") for _ in range(nb)]
    st = [P.sb([128, 4, 6], F32, "st") for _ in range(nb)]
    mv = [P.sb([128, 4], F32, "mv") for _ in range(nb)]
    nxt = moe if (moe is not None and moe.get("route")) else None
    if nxt is not None:
        wr = P.sb([128, 16, NE], F32, "wr")
        P.dma("sp", lambda e: e.dma_start(out=wr[:], in_=nxt["w_r"].rearrange("(k p) e -> p k e", p=128)), writes=[wr])
        brB = P.sb([128, NE], F32, "brB")
        P.dma("sp", lambda e: e.dma_start(out=brB[:], in_=bcast_rows(nxt["b_r"])), writes=[brB])
        xTf = [P.sb([128, 16, 128], F32, "xTf") for _ in range(2)]
        lg = [P.sb([128, NE], F32, "lg") for _ in range(2)]
        t8 = [P.sb([128, 8], F32, "t8") for _ in range(2)]
        msk = [P.sb([128, NE], F32, "msk") for _ in range(2)]
        mskB = [P.sb([128, NE], BF16, "mskB") for _ in range(NT)]
        exs = [P.sb([128, NE], F32, "exs") for _ in range(2)]
        gs = [P.sb([128, 2], F32, "gs") for _ in range(2)]
        posc = [P.sb([128, NE], F32, "posc") for _ in range(2)]
        oh = [P.sb([128, NE], F32, "oh") for _ in range(2)]
        sl_f = [P.sb([128, 4], F32, "slf") for _ in range(2)]
        slots, gates = nxt["slots"], nxt["gates"]

    for it in range(NT):
        j = it % nb
        rows = slice(it * 128, (it + 1) * 128)
        hi, ad, xb_, st_, mv_ = hin[j], add[j], xbt[j], st[j], mv[j]
        P.dma("sp", lambda e, hi=hi, rows=rows: e.dma_start(out=hi[:], in_=h_in[rows, :]), writes=[hi])
        if add_mode == "hbm":
            P.dma("sp", lambda e, ad=ad, rows=rows: e.dma_start(out=ad[:], in_=mix_hbm[rows, :]), writes=[ad])
        else:
            sl_t, g_t = moe["slots"], moe["gates"]
            for k in range(4):
                yk = ytl[k]
                P.op("pool", lambda e, yk=yk: e.memset(yk[:], 0.0), writes=[yk])
                P.dma("pool", lambda e, yk=yk, k=k, it=it: e.indirect_dma_start(
                    out=yk[:], out_offset=None, in_=moe["ybuf"],
                    in_offset=bass.IndirectOffsetOnAxis(ap=sl_t[:, it, k:k + 1], axis=0),
                    bounds_check=BCREG, oob_is_err=False), reads=[sl_t, moe["ybuf_b"]], writes=[yk])
            P.op("dve", lambda e, ad=ad, it=it: e.tensor_scalar(out=ad[:], in0=ytl[0][:], scalar1=g_t[:, it, 0:1],
                 scalar2=None, op0=ALU.mult), reads=[ytl[0], g_t], writes=[ad])
            for k in range(1, 4):
                P.op("dve", lambda e, ad=ad, it=it, k=k: e.scalar_tensor_tensor(out=ad[:], in0=ytl[k][:],
                     scalar=g_t[:, it, k:k + 1], in1=ad[:], op0=ALU.mult, op1=ALU.add),
                     reads=[ytl[k], g_t, ad], writes=[ad])
        P.op("dve", lambda e, hi=hi, ad=ad: e.scalar_tensor_tensor(out=hi[:], in0=hi[:], scalar=float(ALPHA), in1=ad[:],
             op0=ALU.mult, op1=ALU.add), reads=[hi, ad], writes=[hi])
        for c in range(4):
            P.op("dve", lambda e, hi=hi, st_=st_, c=c: e.bn_stats(out=st_[:, c, :], in_=hi[:, c * 512:(c + 1) * 512]),
                 reads=[hi], writes=[st_])
        P.op("dve", lambda e, st_=st_, mv_=mv_: e.bn_aggr(out=mv_[:, 0:2], in_=st_[:].rearrange("p a b -> p (a b)")),
             reads=[st_], writes=[mv_])
        P.op("act", lambda e, mv_=mv_: e.activation(out=mv_[:, 2:3], in_=mv_[:, 1:2], func=AF.Sqrt, bias=C.epsln[:, 0:1]),
             reads=[mv_, C.epsln], writes=[mv_])
        P.op("dve", lambda e, mv_=mv_: e.reciprocal(out=mv_[:, 3:4], in_=mv_[:, 2:3]), reads=[mv_], writes=[mv_])
        P.op("dve", lambda e, hi=hi, mv_=mv_: e.tensor_scalar(out=hi[:], in0=hi[:], scalar1=mv_[:, 0:1], scalar2=mv_[:, 3:4],
             op0=ALU.subtract, op1=ALU.mult), reads=[hi, mv_], writes=[hi])
        P.op("pool", lambda e, hi=hi: e.tensor_tensor(out=hi[:], in0=hi[:], in1=gB[:], op=ALU.mult), reads=[hi, gB], writes=[hi])
        P.op("pool", lambda e, hi=hi: e.tensor_tensor(out=hi[:], in0=hi[:], in1=bB[:], op=ALU.add), reads=[hi, bB], writes=[hi])
        if out_final is not None:
            P.dma("sp", lambda e, hi=hi, rows=rows: e.dma_start(out=out_final[rows, :], in_=hi[:]), reads=[hi])
        else:
            P.dma("sp", lambda e, hi=hi, rows=rows: e.dma_start(out=h_out[rows, :], in_=hi[:]), reads=[hi])
        if hT is not None or nxt is not None:
            P.op("act", lambda e, hi=hi, xb_=xb_: e.copy(out=xb_[:], in_=hi[:]), reads=[hi], writes=[xb_])
        if hT is not None:
            for half in range(2):
                pb = psb(C)
                for q in range(8):
                    kc = half * 8 + q
                    P.op("pe", lambda e, pb=pb, q=q, kc=kc, xb_=xb_: e.transpose(out=pb[:, q * 128:(q + 1) * 128],
                         in_=xb_[:, kc * 128:(kc + 1) * 128], identity=C.identB[:]), reads=[xb_, C.identB], writes=[pb])
                eng = "dve" if half == 0 else "act"
                if eng == "dve":
                    P.op("dve", lambda e, pb=pb, half=half, it=it: e.tensor_copy(
                        out=hT["t"][:, half * 8:(half + 1) * 8, it * 128:(it + 1) * 128],
                        in_=pb[:].rearrange("p (q t) -> p q t", q=8)), reads=[pb], writes=[hT["b"][it]])
                else:
                    P.op("act", lambda e, pb=pb, half=half, it=it: e.copy(
                        out=hT["t"][:, half * 8:(half + 1) * 8, it * 128:(it + 1) * 128],
                        in_=pb[:].rearrange("p (q t) -> p q t", q=8)), reads=[pb], writes=[hT["b"][it]])
        if nxt is not None:
            jj = it % 2
            xT, lg_, t8_, mk, ex, gs_, pc, oh_, slf = xTf[jj], lg[jj], t8[jj], msk[jj], exs[jj], gs[jj], posc[jj], oh[jj], sl_f[jj]
            mB = mskB[it]
            for q4 in range(4):
                pf = psf(C)
                for q in range(4):
                    kc = q4 * 4 + q
                    P.op("pe", lambda e, pf=pf, q=q, kc=kc, hi=hi: e.transpose(out=pf[:, q * 128:(q + 1) * 128],
                         in_=hi[:, kc * 128:(kc + 1) * 128], identity=C.identF[:]), reads=[hi, C.identF], writes=[pf])
                P.op("act", lambda e, pf=pf, q4=q4, xT=xT: e.copy(out=xT[:, q4 * 4:(q4 + 1) * 4, :],
                     in_=pf[:].rearrange("p (q t) -> p q t", q=4)), reads=[pf], writes=[xT])
            pl = psf(C)
            for kc in range(16):
                P.op("pe", lambda e, pl=pl, kc=kc, xT=xT: e.matmul(pl[:, 0:NE], lhsT=xT[:, kc, :], rhs=wr[:, kc, :],
                     start=(kc == 0), stop=(kc == 15)), reads=[xT, wr], writes=[pl])
            P.op("dve", lambda e, pl=pl, lg_=lg_: e.tensor_tensor(out=lg_[:], in0=pl[:, 0:NE], in1=brB[:], op=ALU.add),
                 reads=[pl, brB], writes=[lg_])
            P.op("dve", lambda e, lg_=lg_, t8_=t8_: e.max(out=t8_[:], in_=lg_[:]), reads=[lg_], writes=[t8_])
            P.op("dve", lambda e, lg_=lg_, t8_=t8_, mk=mk: e.tensor_scalar(out=mk[:], in0=lg_[:], scalar1=t8_[:, 3:4], scalar2=None,
                 op0=ALU.is_ge), reads=[lg_, t8_], writes=[mk])
            P.op("dve", lambda e, mk=mk, mB=mB: e.tensor_copy(out=mB[:], in_=mk[:]), reads=[mk], writes=[mB])
            P.op("dve", lambda e, t8_=t8_, gs_=gs_: e.tensor_scalar(out=gs_[:, 0:1], in0=t8_[:, 0:1], scalar1=-1.0, scalar2=None,
                 op0=ALU.mult), reads=[t8_], writes=[gs_])
            P.op("act", lambda e, lg_=lg_, ex=ex, gs_=gs_: e.activation(out=ex[:], in_=lg_[:], func=AF.Exp, bias=gs_[:, 0:1]),
                 reads=[lg_, gs_], writes=[ex])
            P.op("dve", lambda e, ex=ex, mk=mk: e.tensor_tensor(out=ex[:], in0=ex[:], in1=mk[:], op=ALU.mult), reads=[ex, mk], writes=[ex])
            P.op("dve", lambda e, ex=ex, gs_=gs_: e.reduce_sum(out=gs_[:, 1:2], in_=ex[:], axis=AX.X), reads=[ex], writes=[gs_])
            P.op("dve", lambda e, gs_=gs_: e.reciprocal(out=gs_[:, 1:2], in_=gs_[:, 1:2]), reads=[gs_], writes=[gs_])
            P.op("dve", lambda e, ex=ex, gs_=gs_: e.tensor_scalar(out=ex[:], in0=ex[:], scalar1=gs_[:, 1:2], scalar2=None, op0=ALU.mult),
                 reads=[ex, gs_], writes=[ex])
            pp = psf(C)
            P.op("pe", lambda e, pp=pp, mB=mB: e.matmul(pp[:, 0:NE], lhsT=C.triB[:], rhs=mB[:], start=True, stop=(it == 0)),
                 reads=[C.triB, mB], writes=[pp])
            for pt in range(it):
                P.op("pe", lambda e, pp=pp, pt=pt: e.matmul(pp[:, 0:NE], lhsT=C.onesB[:], rhs=mskB[pt][:], start=False, stop=(pt == it - 1)),
                     reads=[C.onesB, mskB[pt]], writes=[pp])
            P.op("dve", lambda e, pp=pp, pc=pc: e.tensor_scalar(out=pc[:], in0=pp[:, 0:NE], scalar1=float(CAP), scalar2=1.0e6,
                 op0=ALU.is_ge, op1=ALU.mult), reads=[pp], writes=[pc])
            P.op("dve", lambda e, pp=pp, pc=pc: e.tensor_tensor(out=pc[:], in0=pc[:], in1=pp[:, 0:NE], op=ALU.add), reads=[pp, pc], writes=[pc])
            P.op("dve", lambda e, pc=pc: e.tensor_tensor(out=pc[:], in0=pc[:], in1=C.eoff[:], op=ALU.add), reads=[pc, C.eoff], writes=[pc])
            for k in range(4):
                P.op("dve", lambda e, k=k, lg_=lg_, t8_=t8_, oh_=oh_: e.tensor_scalar(out=oh_[:], in0=lg_[:], scalar1=t8_[:, k:k + 1],
                     scalar2=None, op0=ALU.is_equal), reads=[lg_, t8_], writes=[oh_])
                P.op("dve", lambda e, k=k, oh_=oh_, pc=pc, mk=mk: e.tensor_tensor(out=mk[:], in0=oh_[:], in1=pc[:], op=ALU.mult),
                     reads=[oh_, pc], writes=[mk])
                P.op("dve", lambda e, k=k, mk=mk, slf=slf: e.reduce_sum(out=slf[:, k:k + 1], in_=mk[:], axis=AX.X), reads=[mk], writes=[slf])
                P.op("dve", lambda e, k=k, oh_=oh_, ex=ex, mk=mk: e.tensor_tensor(out=mk[:], in0=oh_[:], in1=ex[:], op=ALU.mult),
                     reads=[oh_, ex], writes=[mk])
                P.op("dve", lambda e, k=k, mk=mk, it=it: e.reduce_sum(out=gates[:, it, k:k + 1], in_=mk[:], axis=AX.X), reads=[mk], writes=[gates])
            P.op("dve", lambda e, slf=slf, it=it: e.tensor_copy(out=slots[:, it, :], in_=slf[:]), reads=[slf], writes=[slots])
            for k in range(4):
                P.dma("pool", lambda e, k=k, it=it, xb_=xb_: e.indirect_dma_start(
                    out=nxt["xbuf"], out_offset=bass.IndirectOffsetOnAxis(ap=slots[:, it, k:k + 1], axis=0),
                    in_=xb_[:], in_offset=None, bounds_check=BCREG, oob_is_err=False),
                    reads=[xb_, slots], writes=[nxt["xbuf_b"]])


def stage_moe(C, *, moe, w1, b1, w2, b2):
    nc, P = C.nc, C.P
    P.barrier()
    P.reset()
    sb_save = P.sb_off
    P.sb_off = C.hT_off
    NTT = CAP // 128
    NW1, NW2 = 44, 9
    w1r = [P.sb([128, 2, 512], BF16, "w1r") for _ in range(NW1)]
    w2r = [P.sb([128, D], BF16, "w2r") for _ in range(NW2)]
    xtok = [P.sb([128, D], BF16, "xtok") for _ in range(2)]
    xeT = [P.sb([128, 16, CAP], BF16, "xeT") for _ in range(2)]
    actT = [P.sb([128, 8, CAP], BF16, "actT") for _ in range(2)]
    b1t = [P.sb([128, 16], F32, "b1t") for _ in range(2)]
    b1r = [P.sb([16, 128], F32, "b1r") for _ in range(2)]
    b2B = [P.sb([128, D], F32, "b2B") for _ in range(2)]
    gq = [P.sb([128, CAP], F32, "gq") for _ in range(2)]
    sg = [P.sb([128, CAP], F32, "sg") for _ in range(2)]
    uq = [P.sb([128, CAP], F32, "uq") for _ in range(2)]
    yst = [P.sb([128, 512], F32, "yst") for _ in range(4)]
    xbuf, ybuf = moe["xbuf"], moe["ybuf"]
    i1 = i2 = iy = 0
    def gather_T(ex, j):
            for tt in range(NTT):
                xt = xtok[tt % 2]
                P.dma("sp", lambda e, xt=xt, ex=ex, tt=tt: e.dma_start(out=xt[:], in_=xbuf[ex * CAP + tt * 128: ex * CAP + (tt + 1) * 128, :]),
                      reads=[moe["xbuf_b"]], writes=[xt])
                for half in range(2):
                    pb = psb(C)
                    for q in range(8):
                        kc = half * 8 + q
                        P.op("pe", lambda e, pb=pb, q=q, kc=kc, xt=xt: e.transpose(out=pb[:, q * 128:(q + 1) * 128],
                             in_=xt[:, kc * 128:(kc + 1) * 128], identity=C.identB[:]), reads=[xt, C.identB], writes=[pb])
                    P.op("dve" if half == 0 else "act", (lambda e, pb=pb, half=half, tt=tt, j=j: e.tensor_copy(
                        out=xeT[j][:, half * 8:(half + 1) * 8, tt * 128:(tt + 1) * 128], in_=pb[:].rearrange("p (q t) -> p q t", q=8)))
                        if half == 0 else (lambda e, pb=pb, half=half, tt=tt, j=j: e.copy(
                            out=xeT[j][:, half * 8:(half + 1) * 8, tt * 128:(tt + 1) * 128], in_=pb[:].rearrange("p (q t) -> p q t", q=8))),
                        reads=[pb], writes=[xeT[j]])
    for ex in range(NE):
        j = ex % 2
        w1s = [[None] * 16 for _ in range(2)]
        for hf in range(2):
            for kc in range(16):
                t = w1r[i1 % NW1]; i1 += 1
                w1s[hf][kc] = t
                P.dma("pool", lambda e, t=t, ex=ex, kc=kc, hf=hf: e.dma_start(out=t[:],
                      in_=w1[ex, kc * 128:(kc + 1) * 128, :].rearrange("p (two c) -> p two c", two=2)[:, :, hf * 512:(hf + 1) * 512]), writes=[t])
        w2s = []
        for f in range(8):
            t = w2r[i2 % NW2]; i2 += 1
            w2s.append(t)
            P.dma("pool", lambda e, t=t, ex=ex, f=f: e.dma_start(out=t[:], in_=w2[ex, f * 128:(f + 1) * 128, :]), writes=[t])
        P.dma("sp", lambda e, j=j, ex=ex: e.dma_start(out=b1r[j][:], in_=b1[ex].rearrange("(c p) -> c p", p=128)), writes=[b1r[j]])
        pbt = psf(C)
        P.op("pe", lambda e, j=j, pbt=pbt: e.transpose(out=pbt[:, 0:16], in_=b1r[j][:], identity=C.identF[0:16, 0:16]),
             reads=[b1r[j], C.identF], writes=[pbt])
        P.op("act", lambda e, j=j, pbt=pbt: e.copy(out=b1t[j][:], in_=pbt[:, 0:16]), reads=[pbt], writes=[b1t[j]])
        P.dma("sp", lambda e, j=j, ex=ex: e.dma_start(out=b2B[j][:], in_=bcast_rows(b2[ex:ex + 1, :])), writes=[b2B[j]])
        if ex == 0:
            gather_T(0, 0)
        for f in range(8):
            pg, pu = psf(C), psf(C)
            hf, fl = f // 4, f % 4
            for kc in range(16):
                P.op("pe", lambda e, pg=pg, kc=kc, fl=fl, j=j, w=w1s[hf][kc]: e.matmul(pg[:, 0:CAP], lhsT=w[:, 0, fl * 128:(fl + 1) * 128],
                     rhs=xeT[j][:, kc, :], start=(kc == 0), stop=(kc == 15)), reads=[w1s[hf][kc], xeT[j]], writes=[pg])
            for kc in range(16):
                P.op("pe", lambda e, pu=pu, kc=kc, fl=fl, j=j, w=w1s[hf][kc]: e.matmul(pu[:, 0:CAP], lhsT=w[:, 1, fl * 128:(fl + 1) * 128],
                     rhs=xeT[j][:, kc, :], start=(kc == 0), stop=(kc == 15)), reads=[w1s[hf][kc], xeT[j]], writes=[pu])
            jj = f % 2
            g_, s_, u_ = gq[jj], sg[jj], uq[jj]
            P.op("dve", lambda e, pg=pg, g_=g_, j=j, f=f: e.tensor_scalar(out=g_[:], in0=pg[:, 0:CAP], scalar1=b1t[j][:, f:f + 1], scalar2=7.0,
                 op0=ALU.add, op1=ALU.min), reads=[pg, b1t[j]], writes=[g_])
            P.op("act", lambda e, g_=g_, s_=s_: e.activation(out=s_[:], in_=g_[:], func=AF.Sigmoid, scale=1.702), reads=[g_], writes=[s_])
            P.op("dve", lambda e, pu=pu, u_=u_, j=j, f=f: e.tensor_scalar(out=u_[:], in0=pu[:, 0:CAP], scalar1=b1t[j][:, 8 + f:9 + f], scalar2=-7.0,
                 op0=ALU.add, op1=ALU.max), reads=[pu, b1t[j]], writes=[u_])
            P.op("dve", lambda e, u_=u_: e.tensor_scalar(out=u_[:], in0=u_[:], scalar1=7.0, scalar2=1.0, op0=ALU.min, op1=ALU.add),
                 reads=[u_], writes=[u_])
            P.op("dve", lambda e, g_=g_, s_=s_: e.tensor_tensor(out=g_[:], in0=g_[:], in1=s_[:], op=ALU.mult), reads=[g_, s_], writes=[g_])
            P.op("dve", lambda e, g_=g_, u_=u_, j=j, f=f: e.tensor_tensor(out=actT[j][:, f, :], in0=g_[:], in1=u_[:], op=ALU.mult),
                 reads=[g_, u_], writes=[actT[j]])
        if ex + 1 < NE:
            gather_T(ex + 1, (ex + 1) % 2)
        for tt in range(NTT):
            for dc in range(4):
                py = psf(C)
                for f in range(8):
                    P.op("pe", lambda e, py=py, f=f, tt=tt, dc=dc, j=j, w=w2s[f]: e.matmul(py[:], lhsT=actT[j][:, f, tt * 128:(tt + 1) * 128],
                         rhs=w[:, dc * 512:(dc + 1) * 512], start=(f == 0), stop=(f == 7)), reads=[actT[j], w2s[f]], writes=[py])
                ys = yst[iy % 4]; iy += 1
                P.op("dve", lambda e, py=py, ys=ys, dc=dc, j=j: e.tensor_tensor(out=ys[:], in0=py[:], in1=b2B[j][:, dc * 512:(dc + 1) * 512], op=ALU.add),
                     reads=[py, b2B[j]], writes=[ys])
                P.dma("sp", lambda e, ys=ys, ex=ex, tt=tt, dc=dc: e.dma_start(
                    out=ybuf[ex * CAP + tt * 128: ex * CAP + (tt + 1) * 128, dc * 512:(dc + 1) * 512], in_=ys[:]),
                    reads=[ys], writes=[moe["ybuf_b"]])
    P.barrier()
    P.sb_off = sb_save


def alloc_persist(nc, P, C):
    setup_common(nc, P, C)
    C.slots = P.sb([128, NT, 4], U32, "slots")
    C.gates = P.sb([128, NT, 4], F32, "gates")
    C.hT_off = (P.sb_off + 63) // 64 * 64
    C.hT = {"t": P.sb([128, 16, S], BF16, "hT"), "b": [Buf() for _ in range(NT)]}
    P.mark()


def new_moe(nc, C, tag, kind="Internal"):
    xbuf = nc.dram_tensor(f"xbuf{tag}", [NE * CAP, D], BF16, kind=kind).ap()
    ybuf = nc.dram_tensor(f"ybuf{tag}", [NE * CAP, D], F32, kind=kind).ap()
    return {"xbuf": xbuf, "ybuf": ybuf, "xbuf_b": Buf(), "ybuf_b": Buf(), "slots": C.slots, "gates": C.gates}


def stage_load_hT(C, h_hbm):
    P = C.P
    P.barrier()
    P.reset()
    hin = [P.sb([128, D], F32, "lh") for _ in range(2)]
    xbt = [P.sb([128, D], BF16, "lxb") for _ in range(2)]
    for it in range(NT):
        hi, xb_ = hin[it % 2], xbt[it % 2]
        P.dma("sp", lambda e: e.dma_start(out=hi[:], in_=h_hbm[it * 128:(it + 1) * 128, :]), writes=[hi])
        P.op("act", lambda e: e.copy(out=xb_[:], in_=hi[:]), reads=[hi], writes=[xb_])
        transpose_to(C, src=xb_, nchunks=16, dst_fn=lambda k0, k1: C.hT["t"][:, k0:k1, it * 128:(it + 1) * 128],
                     dst_buf=C.hT["b"][it])


def transpose_to(C, *, src, nchunks, dst_fn, dst_buf, rows=128):
    P = C.P
    k0 = 0
    while k0 < nchunks:
        k1 = min(k0 + 8, nchunks)
        pb = psb(C)
        for q in range(k1 - k0):
            kc = k0 + q
            P.op("pe", lambda e: e.transpose(out=pb[:, q * 128:(q + 1) * 128], in_=src[:, kc * 128:(kc + 1) * 128],
                 identity=C.identB[:]), reads=[src, C.identB], writes=[pb])
        n = k1 - k0
        if (k0 // 8) % 2 == 0:
            P.op("dve", lambda e: e.tensor_copy(out=dst_fn(k0, k1), in_=pb[:, 0:n * 128].rearrange("p (q t) -> p q t", q=n)),
                 reads=[pb], writes=[dst_buf])
        else:
            P.op("act", lambda e: e.copy(out=dst_fn(k0, k1), in_=pb[:, 0:n * 128].rearrange("p (q t) -> p q t", q=n)),
                 reads=[pb], writes=[dst_buf])
        k0 = k1


TWO_PI = float(2 * np.pi)


def build_rope_tables(C):
    P = C.P
    C.cosT = P.sb([128, NT, 32], F32, "cosT")
    C.sinT = P.sb([128, NT, 32], F32, "sinT")
    C.cosF = P.sb([64, S], F32, "cosF")
    C.sinF = P.sb([64, S], F32, "sinF")
    C.negpi = P.sb([128, 1], F32, "negpi")
    P.op("pool", lambda e: e.memset(C.negpi[:], -float(np.pi)), writes=[C.negpi])

    def sincos(dst_sin, dst_cos, ang, tmp, tmpi, shape_p):
        for dst, shift in ((dst_sin, 0.0), (dst_cos, float(np.pi / 2))):
            P.op("dve", lambda e: e.tensor_scalar(out=tmp, in0=ang, scalar1=shift, scalar2=1.0 / TWO_PI, op0=ALU.add, op1=ALU.mult),
                 reads=[C.rt_b], writes=[C.rt_b])
            P.op("dve", lambda e: e.tensor_copy(out=tmpi, in_=tmp), reads=[C.rt_b], writes=[C.rt_b])
            P.op("dve", lambda e: e.tensor_copy(out=tmp, in_=tmpi), reads=[C.rt_b], writes=[C.rt_b])
            P.op("dve", lambda e: e.tensor_scalar(out=tmp, in0=tmp, scalar1=-TWO_PI, scalar2=shift, op0=ALU.mult, op1=ALU.add),
                 reads=[C.rt_b], writes=[C.rt_b])
            P.op("dve", lambda e: e.tensor_tensor(out=tmp, in0=tmp, in1=ang, op=ALU.add), reads=[C.rt_b], writes=[C.rt_b])
            for (cmp, thr, corr) in ((ALU.is_gt, float(np.pi), -TWO_PI), (ALU.is_lt, -float(np.pi), TWO_PI)):
                P.op("dve", lambda e: e.tensor_scalar(out=dst, in0=tmp, scalar1=thr, scalar2=corr, op0=cmp, op1=ALU.mult),
                     reads=[C.rt_b], writes=[C.rt_b])
                P.op("dve", lambda e: e.tensor_tensor(out=tmp, in0=tmp, in1=dst, op=ALU.add), reads=[C.rt_b], writes=[C.rt_b])
            P.op("dve", lambda e: e.tensor_scalar(out=tmp, in0=tmp, scalar1=float(np.pi), scalar2=-float(np.pi), op0=ALU.min, op1=ALU.max),
                 reads=[C.rt_b], writes=[C.rt_b])
            P.op("act", lambda e: e.activation(out=dst, in_=tmp, func=AF.Sin), reads=[C.rt_b], writes=[C.rt_b])

    C.rt_b = Buf()
    _after_tables = P.sb_off
    posT = P.sb([128, NT], F32, "posT")
    invT = P.sb([128, 32], F32, "invT")
    angT = P.sb([128, NT, 32], F32, "angT")
    tmpT = P.sb([128, NT, 32], F32, "tmpT")
    tmpTi = P.sb([128, NT, 32], I32, "tmpTi")
    P.op("pool", lambda e: e.iota(posT[:], pattern=[[128, NT]], base=0, channel_multiplier=1, allow_small_or_imprecise_dtypes=True),
         writes=[C.rt_b])
    P.op("pool", lambda e: e.iota(invT[:], pattern=[[1, 32]], base=0, channel_multiplier=0, allow_small_or_imprecise_dtypes=True),
         reads=[C.rt_b], writes=[C.rt_b])
    P.op("act", lambda e: e.activation(out=invT[:], in_=invT[:], func=AF.Exp, scale=-float(np.log(10000.0) / 32.0)),
         reads=[C.rt_b], writes=[C.rt_b])
    for it in range(NT):
        P.op("dve", lambda e: e.tensor_scalar(out=angT[:, it, :], in0=invT[:], scalar1=posT[:, it:it + 1], scalar2=None, op0=ALU.mult),
             reads=[C.rt_b], writes=[C.rt_b])
    f3 = lambda t: t[:].rearrange("p a b -> p (a b)")
    sincos(f3(C.sinT), f3(C.cosT), f3(angT), f3(tmpT), f3(tmpTi), 128)
    posF = P.sb([64, S], F32, "posF")
    invF = P.sb([64, 1], F32, "invF")
    tmpF = P.sb([64, S], F32, "tmpF")
    tmpFi = P.sb([64, S], I32, "tmpFi")
    P.op("pool", lambda e: e.iota(posF[:], pattern=[[1, S]], base=0, channel_multiplier=0, allow_small_or_imprecise_dtypes=True),
         reads=[C.rt_b], writes=[C.rt_b])
    for half in range(2):
        P.op("pool", lambda e: e.iota(invF[half * 32:(half + 1) * 32, :], pattern=[[0, 1]], base=0, channel_multiplier=1,
             allow_small_or_imprecise_dtypes=True), reads=[C.rt_b], writes=[C.rt_b])
    P.op("act", lambda e: e.activation(out=invF[:], in_=invF[:], func=AF.Exp, scale=-float(np.log(10000.0) / 32.0)),
         reads=[C.rt_b], writes=[C.rt_b])
    P.op("dve", lambda e: e.tensor_scalar(out=posF[:], in0=posF[:], scalar1=invF[:, 0:1], scalar2=None, op0=ALU.mult),
         reads=[C.rt_b], writes=[C.rt_b])
    sincos(C.sinF[:], C.cosF[:], posF[:], tmpF[:], tmpFi[:], 64)
    P.op("dve", lambda e: e.tensor_scalar(out=C.sinF[0:32, :], in0=C.sinF[0:32, :], scalar1=-1.0, scalar2=None, op0=ALU.mult),
         reads=[C.rt_b], writes=[C.rt_b])
    P.barrier()
    P.sb_off = _after_tables


def stage_outproj(C, *, srcs, w_out, mix_hbm):
    P = C.P
    nsrc = len(srcs)
    wbufs = [[P.sb([128, 512], BF16, "wo") for _ in range(nsrc)] for _ in range(2)]
    ost = [P.sb([128, 512], F32, "ost") for _ in range(3)]
    io = 0
    for cc in range(4):
        wb = wbufs[cc % 2]
        for i, (fn, K, row0, buf) in enumerate(srcs):
            P.dma("pool", lambda e: e.dma_start(out=wb[i][0:K, :], in_=w_out[row0:row0 + K, cc * 512:(cc + 1) * 512]), writes=[wb[i]])
        for it in range(NT):
            py = psf(C)
            for i, (fn, K, row0, buf) in enumerate(srcs):
                P.op("pe", lambda e: e.matmul(py[:], lhsT=fn(it), rhs=wb[i][0:K, :], start=(i == 0), stop=(i == nsrc - 1)),
                     reads=[buf, wb[i]], writes=[py])
            o = ost[io % 3]; io += 1
            if io % 2:
                P.op("act", lambda e: e.copy(out=o[:], in_=py[:]), reads=[py], writes=[o])
            else:
                P.op("dve", lambda e: e.tensor_copy(out=o[:], in_=py[:]), reads=[py], writes=[o])
            P.dma("sp", lambda e: e.dma_start(out=mix_hbm[it * 128:(it + 1) * 128, cc * 512:(cc + 1) * 512], in_=o[:]), reads=[o])


MLA_SCALE = float(192 ** -0.5)


def stage_mla(C, *, w_in, q_norm, kv_norm, w_uq, w_ukv, w_out, mix_hbm, dbg_o=None):
    nc, P = C.nc, C.P
    P.barrier()
    P.reset()
    build_rope_tables(C)
    hT = C.hT
    cqnT = P.sb([128, 4, S], BF16, "cqnT"); cq_b = [Buf() for _ in range(NT)]
    ckvT = P.sb([128, 4, S], BF16, "ckvT"); ckv_b = [Buf() for _ in range(NT)]
    krT = P.sb([64, S], BF16, "krT"); kr_b = [Buf() for _ in range(NT)]
    _save = P.sb_off
    P.sb_off = C.hT_off
    oT = P.sb([128, 16, S], BF16, "oT"); oT_b = [Buf() for _ in range(16)]
    P.sb_off = _save
    triI = P.sb([128, 128], BF16, "triI")
    onesF = P.sb([1, 128], F32, "onesF")
    onesK = P.sb([128, 1], BF16, "onesK")
    P.op("pool", lambda e: e.memset(triI[:], 1.0), writes=[triI])
    P.op("pool", lambda e: e.affine_select(out=triI[:], in_=triI[:], pattern=[[1, 128]], compare_op=ALU.is_ge, fill=0.0,
         base=0, channel_multiplier=-1), reads=[triI], writes=[triI])
    P.op("pool", lambda e: e.memset(onesF[:], 1.0), writes=[onesF])
    P.op("pool", lambda e: e.memset(onesK[:], 1.0), writes=[onesK])
    mark1 = P.sb_off
    win = P.sb([128, 16, 1088], BF16, "win")
    for kc4 in range(4):
        P.dma("pool", lambda e: e.dma_start(out=win[:, kc4 * 4:(kc4 + 1) * 4, :],
              in_=w_in[kc4 * 512:(kc4 + 1) * 512, :].rearrange("(k p) c -> p k c", p=128)), writes=[win])
    gq = P.sb([128, 512], F32, "gq"); gkv = P.sb([128, 512], F32, "gkv")
    P.dma("sp", lambda e: e.dma_start(out=gq[:], in_=bcast_rows(q_norm)), writes=[gq])
    P.dma("sp", lambda e: e.dma_start(out=gkv[:], in_=bcast_rows(kv_norm)), writes=[gkv])
    epsr = P.sb([128, 1], F32, "epsr")
    P.op("pool", lambda e: e.memset(epsr[:], 1e-6), writes=[epsr])
    junk = [P.sb([128, 512], F32, "junk") for _ in range(2)]
    ss = [P.sb([128, 4], F32, "ss") for _ in range(2)]
    cn = [P.sb([128, 512], BF16, "cn") for _ in range(2)]
    krf = [P.sb([128, 64], F32, "krf") for _ in range(2)]
    krt = [P.sb([128, 4, 32], F32, "krt") for _ in range(2)]
    krb = [P.sb([128, 128], BF16, "krb") for _ in range(2)]
    ic = 0
    for it in range(NT):
        for which, (c0, gB, dstT, dst_b) in enumerate(((0, gq, cqnT, cq_b), (512, gkv, ckvT, ckv_b))):
            pc = psf(C)
            for kc in range(16):
                P.op("pe", lambda e: e.matmul(pc[:], lhsT=hT["t"][:, kc, it * 128:(it + 1) * 128], rhs=win[:, kc, c0:c0 + 512],
                     start=(kc == 0), stop=(kc == 15)), reads=[hT["b"][it], win], writes=[pc])
            jk, s_, cn_ = junk[ic % 2], ss[ic % 2], cn[ic % 2]; ic += 1
            P.op("act", lambda e: e.activation(out=jk[:], in_=pc[:], func=AF.Square, accum_out=s_[:, 0:1]), reads=[pc], writes=[jk, s_])
            P.op("act", lambda e: e.activation(out=s_[:, 1:2], in_=s_[:, 0:1], func=AF.Sqrt, scale=1.0 / 512.0, bias=epsr[:, 0:1]),
                 reads=[s_, epsr], writes=[s_])
            P.op("dve", lambda e: e.reciprocal(out=s_[:, 2:3], in_=s_[:, 1:2]), reads=[s_], writes=[s_])
            P.op("dve", lambda e: e.scalar_tensor_tensor(out=cn_[:], in0=pc[:], scalar=s_[:, 2:3], in1=gB[:], op0=ALU.mult, op1=ALU.mult),
                 reads=[pc, s_, gB], writes=[cn_])
            transpose_to(C, src=cn_, nchunks=4, dst_fn=lambda k0, k1: dstT[:, k0:k1, it * 128:(it + 1) * 128], dst_buf=dst_b[it])
        pk = psf(C)
        for kc in range(16):
            P.op("pe", lambda e: e.matmul(pk[:, 0:64], lhsT=hT["t"][:, kc, it * 128:(it + 1) * 128], rhs=win[:, kc, 1024:1088],
                 start=(kc == 0), stop=(kc == 15)), reads=[hT["b"][it], win], writes=[pk])
        kf, kt_, kb = krf[it % 2], krt[it % 2], krb[it % 2]
        P.op("act", lambda e: e.copy(out=kf[:], in_=pk[:, 0:64]), reads=[pk], writes=[kf])
        cs, sn = C.cosT[:, it, :], C.sinT[:, it, :]
        P.op("dve", lambda e: e.tensor_tensor(out=kt_[:, 0, :], in0=kf[:, 0:32], in1=cs, op=ALU.mult), reads=[kf, C.rt_b], writes=[kt_])
        P.op("dve", lambda e: e.tensor_tensor(out=kt_[:, 1, :], in0=kf[:, 32:64], in1=sn, op=ALU.mult), reads=[kf, C.rt_b], writes=[kt_])
        P.op("dve", lambda e: e.tensor_tensor(out=kt_[:, 2, :], in0=kf[:, 0:32], in1=sn, op=ALU.mult), reads=[kf, C.rt_b], writes=[kt_])
        P.op("dve", lambda e: e.tensor_tensor(out=kt_[:, 3, :], in0=kf[:, 32:64], in1=cs, op=ALU.mult), reads=[kf, C.rt_b], writes=[kt_])
        P.op("pool", lambda e: e.memset(kb[:], 0.0), writes=[kb])
        P.op("dve", lambda e: e.tensor_tensor(out=kb[:, 0:32], in0=kt_[:, 0, :], in1=kt_[:, 1, :], op=ALU.subtract), reads=[kt_], writes=[kb])
        P.op("dve", lambda e: e.tensor_tensor(out=kb[:, 32:64], in0=kt_[:, 2, :], in1=kt_[:, 3, :], op=ALU.add), reads=[kt_], writes=[kb])
        pb = psb(C)
        P.op("pe", lambda e: e.transpose(out=pb[:, 0:128], in_=kb[:], identity=C.identB[:]), reads=[kb, C.identB], writes=[pb])
        P.op("act", lambda e: e.copy(out=krT[:, it * 128:(it + 1) * 128], in_=pb[0:64, 0:128]), reads=[pb], writes=[kr_b[it]])
    P.barrier()
    P.sb_off = mark1
    NB = 2
    wq = [P.sb([128, 4, 192], BF16, "wq") for _ in range(NB)]
    wqs = [P.sb([128, 4, 64], BF16, "wqs") for _ in range(NB)]
    wkv = [P.sb([128, 4, 256], BF16, "wkv") for _ in range(NB)]
    qnT = [P.sb([128, S], BF16, "qnT") for _ in range(NB)]
    qrT = [P.sb([64, S], BF16, "qrT") for _ in range(NB)]
    knT = [P.sb([128, S], BF16, "knT") for _ in range(NB)]
    vh = [P.sb([128, NT, 128], BF16, "vh") for _ in range(NB)]
    t1 = [P.sb([64, 512], F32, "t1") for _ in range(2)]
    t2 = [P.sb([64, 512], F32, "t2") for _ in range(2)]
    pT = [P.sb([128, 512], BF16, "pT") for _ in range(4)]
    osb = [P.sb([128, 512], F32, "osb") for _ in range(2)]
    rden = [P.sb([1, 512], F32, "rden") for _ in range(2)]
    ip = 0
    allc = cq_b + ckv_b + kr_b
    for h in range(16):
        j = h % NB
        P.dma("pool", lambda e: e.dma_start(out=wq[j][:], in_=w_uq[:, h * 192:(h + 1) * 192].rearrange("(k p) c -> p k c", p=128)), writes=[wq[j]])
        P.dma("pool", lambda e: e.dma_start(out=wkv[j][:], in_=w_ukv[:, h * 256:(h + 1) * 256].rearrange("(k p) c -> p k c", p=128)), writes=[wkv[j]])
        P.op("dve", lambda e: e.tensor_copy(out=wqs[j][:, :, 0:32], in_=wq[j][:, :, 160:192]), reads=[wq[j]], writes=[wqs[j]])
        P.op("dve", lambda e: e.tensor_copy(out=wqs[j][:, :, 32:64], in_=wq[j][:, :, 128:160]), reads=[wq[j]], writes=[wqs[j]])
        for g in range(4):
            ts = slice(g * 512, (g + 1) * 512)
            pq = psf(C)
            for kc in range(4):
                P.op("pe", lambda e: e.matmul(pq[:], lhsT=wq[j][:, kc, 0:128], rhs=cqnT[:, kc, ts], start=(kc == 0), stop=(kc == 3)),
                     reads=[wq[j]] + cq_b[g * 4:(g + 1) * 4], writes=[pq])
            P.op("act", lambda e: e.copy(out=qnT[j][:, ts], in_=pq[:]), reads=[pq], writes=[qnT[j]])
            pa, pb_ = psf(C), psf(C)
            for kc in range(4):
                P.op("pe", lambda e: e.matmul(pa[0:64, :], lhsT=wq[j][:, kc, 128:192], rhs=cqnT[:, kc, ts], start=(kc == 0), stop=(kc == 3)),
                     reads=[wq[j]] + cq_b[g * 4:(g + 1) * 4], writes=[pa])
            for kc in range(4):
                P.op("pe", lambda e: e.matmul(pb_[0:64, :], lhsT=wqs[j][:, kc, :], rhs=cqnT[:, kc, ts], start=(kc == 0), stop=(kc == 3)),
                     reads=[wqs[j]] + cq_b[g * 4:(g + 1) * 4], writes=[pb_])
            a1, a2 = t1[g % 2], t2[g % 2]
            P.op("dve", lambda e: e.tensor_tensor(out=a1[:], in0=pa[0:64, :], in1=C.cosF[:, ts], op=ALU.mult), reads=[pa, C.rt_b], writes=[a1])
            P.op("dve", lambda e: e.tensor_tensor(out=a2[:], in0=pb_[0:64, :], in1=C.sinF[:, ts], op=ALU.mult), reads=[pb_, C.rt_b], writes=[a2])
            P.op("pool", lambda e: e.tensor_tensor(out=qrT[j][:, ts], in0=a1[:], in1=a2[:], op=ALU.add), reads=[a1, a2], writes=[qrT[j]])
            pk = psf(C)
            for kc in range(4):
                P.op("pe", lambda e: e.matmul(pk[:], lhsT=wkv[j][:, kc, 0:128], rhs=ckvT[:, kc, ts], start=(kc == 0), stop=(kc == 3)),
                     reads=[wkv[j]] + ckv_b[g * 4:(g + 1) * 4], writes=[pk])
            P.op("act", lambda e: e.copy(out=knT[j][:, ts], in_=pk[:]), reads=[pk], writes=[knT[j]])
        for t4 in range(4):
            pv = psf(C)
            for q in range(4):
                it = t4 * 4 + q
                for kc in range(4):
                    P.op("pe", lambda e: e.matmul(pv[:, q * 128:(q + 1) * 128], lhsT=ckvT[:, kc, it * 128:(it + 1) * 128], rhs=wkv[j][:, kc, 128:256],
                         start=(kc == 0), stop=(kc == 3)), reads=[wkv[j], ckv_b[it]], writes=[pv])
            P.op("dve", lambda e: e.tensor_copy(out=vh[j][:, t4 * 4:(t4 + 1) * 4, :], in_=pv[:].rearrange("p (q d) -> p q d", q=4)),
                 reads=[pv], writes=[vh[j]])
        for g in range(4):
            po, pd = C.psF[4], C.psF[5]
            nkt = 4 * g + 4
            for kt in range(nkt):
                i = kt - 4 * g
                q0 = g * 512 + (128 * i if i > 0 else 0)
                n = (g + 1) * 512 - q0
                c0 = q0 - g * 512
                ps_ = psf(C, (0, 1, 2))
                P.op("pe", lambda e: e.matmul(ps_[:, 0:n], lhsT=knT[j][:, kt * 128:(kt + 1) * 128], rhs=qnT[j][:, q0:q0 + n], start=True, stop=False),
                     reads=[knT[j], qnT[j]], writes=[ps_])
                P.op("pe", lambda e: e.matmul(ps_[:, 0:n], lhsT=krT[:, kt * 128:(kt + 1) * 128], rhs=qrT[j][:, q0:q0 + n], start=False, stop=True),
                     reads=[kr_b[kt], qrT[j]], writes=[ps_])
                p_ = pT[ip % 4]; ip += 1
                P.op("act", lambda e: e.activation(out=p_[:, 0:n], in_=ps_[:, 0:n], func=AF.Exp, scale=MLA_SCALE), reads=[ps_], writes=[p_])
                if i >= 0:
                    P.op("pool", lambda e: e.tensor_tensor(out=p_[:, 0:128], in0=p_[:, 0:128], in1=triI[:], op=ALU.mult), reads=[p_, triI], writes=[p_])
                P.op("pe", lambda e: e.matmul(po[:, c0:512], lhsT=vh[j][:, kt, :], rhs=p_[:, 0:n], start=(kt == 0), stop=(kt == nkt - 1)),
                     reads=[vh[j], p_], writes=[po])
                P.op("pe", lambda e: e.matmul(pd[0:1, c0:512], lhsT=onesK[:], rhs=p_[:, 0:n], start=(kt == 0), stop=(kt == nkt - 1)),
                     reads=[onesK, p_], writes=[pd])
            ob, rd = osb[g % 2], rden[g % 2]
            P.op("act", lambda e: e.copy(out=ob[:], in_=po[:]), reads=[po], writes=[ob])
            P.op("dve", lambda e: e.reciprocal(out=rd[:], in_=pd[0:1, :]), reads=[pd], writes=[rd])
            pbc = C.psF[3]
            P.op("pe", lambda e: e.matmul(pbc[:], lhsT=onesF[:], rhs=rd[:], start=True, stop=True), reads=[onesF, rd], writes=[pbc])
            P.op("dve", lambda e: e.tensor_tensor(out=oT[:, h, g * 512:(g + 1) * 512], in0=ob[:], in1=pbc[:], op=ALU.mult),
                 reads=[ob, pbc], writes=[oT_b[h]])
    if dbg_o is not None:
        for h in range(16):
            P.dma("sp", lambda e: e.dma_start(out=dbg_o[h], in_=oT[:, h, :]), reads=[oT_b[h]])
    P.barrier()
    P.sb_off = mark1
    srcs = [((lambda it, h=h: oT[:, h, it * 128:(it + 1) * 128]), 128, h * 128, oT_b[h]) for h in range(16)]
    stage_outproj(C, srcs=srcs, w_out=w_out, mix_hbm=mix_hbm)


MOBA_SCALE = 0.125
BIGNEG = 30000.0


def stage_moba(C, *, w_in, mixT_hbm):
    nc, P = C.nc, C.P
    P.barrier()
    P.reset()
    hT = C.hT
    allh = hT["b"]
    biasBase = P.sb([128, 16], F32, "biasBase")
    cqBase = P.sb([128, 4], F32, "cqBase")
    nmc = P.sb([128, 16, 8], F32, "nmc")
    notpast = P.sb([128, 16, 8], F32, "notpast")
    negfill = P.sb([128, 16, 8], F32, "negfill")
    triI = P.sb([128, 128], BF16, "triI")
    onesF = P.sb([1, 128], F32, "onesF")
    onesK = P.sb([128, 1], BF16, "onesK")
    P.op("pool", lambda e: e.iota(biasBase[:], pattern=[[128, 16]], base=-2047, channel_multiplier=1, allow_small_or_imprecise_dtypes=True), writes=[biasBase])
    P.op("pool", lambda e: e.iota(cqBase[:], pattern=[[-128, 4]], base=511, channel_multiplier=-1, allow_small_or_imprecise_dtypes=True), writes=[cqBase])
    P.op("pool", lambda e: e.iota(nmc[:], pattern=[[-1, 8], [0, 2], [1, 8]], base=0, channel_multiplier=0, allow_small_or_imprecise_dtypes=True), writes=[nmc])
    P.op("dve", lambda e: e.tensor_scalar(out=notpast[:], in0=nmc[:], scalar1=0.0, scalar2=None, op0=ALU.is_ge), reads=[nmc], writes=[notpast])
    P.op("dve", lambda e: e.tensor_scalar(out=negfill[:], in0=notpast[:], scalar1=-1.0e30, scalar2=None, op0=ALU.mult), reads=[notpast], writes=[negfill])
    triN = P.sb([128, 128], BF16, "triN")
    P.op("pool", lambda e: e.memset(triN[:], 0.0), writes=[triN])
    P.op("pool", lambda e: e.affine_select(out=triN[:], in_=triN[:], pattern=[[1, 128]], compare_op=ALU.is_ge, fill=-8.0 * BIGNEG,
         base=0, channel_multiplier=-1), reads=[triN], writes=[triN])
    P.op("pool", lambda e: e.memset(onesF[:], 1.0), writes=[onesF])
    P.op("pool", lambda e: e.memset(onesK[:], 1.0), writes=[onesK])
    NB = 2
    wq4 = [P.sb([128, 16, 256], BF16, "wq4") for _ in range(2)]
    wk4 = [P.sb([128, 16, 256], BF16, "wk4") for _ in range(2)]
    wv4 = [P.sb([128, 16, 256], BF16, "wv4") for _ in range(2)]
    QA = [P.sb([72, S], BF16, "QA") for _ in range(NB)]
    KA = [P.sb([72, S], BF16, "KA") for _ in range(NB)]
    VA = [P.sb([128, NT, 64], BF16, "VA") for _ in range(NB)]
    Xpad = [P.sb([128, NT, 128], BF16, "Xpad") for _ in range(NB)]
    km = [P.sb([64, 8], F32, "km") for _ in range(NB)]
    kmB = [P.sb([64, 8], BF16, "kmB") for _ in range(NB)]
    gm = [P.sb([128, 16, 8], F32, "gm") for _ in range(NB)]
    cmp = [P.sb([128, 16, 8, 8], F32, "cmp") for _ in range(1)]
    rank = [P.sb([128, 16, 8], F32, "rank") for _ in range(NB)]
    cqh = [P.sb([128, 4], F32, "cqh") for _ in range(NB)]
    bk = [P.sb([128, 16], F32, "bk") for _ in range(NB)]
    pT = [P.sb([128, 512], BF16, "pT") for _ in range(4)]
    osb = [P.sb([64, 512], F32, "osb") for _ in range(2)]
    rden = [P.sb([1, 512], F32, "rden") for _ in range(2)]
    aS = [P.sb([64, S], BF16, "aS") for _ in range(NB)]
    for j in range(NB):
        P.op("pool", lambda e: e.memset(KA[j][64:72, :], 1.0), writes=[KA[j]])
        P.op("pool", lambda e: e.affine_select(out=KA[j][64:72, :], in_=KA[j][64:72, :], pattern=[[1, S]], compare_op=ALU.is_ge, fill=0.0,
             base=0, channel_multiplier=-256), reads=[KA[j]], writes=[KA[j]])
        P.op("pool", lambda e: e.affine_select(out=KA[j][64:72, :], in_=KA[j][64:72, :], pattern=[[-1, S]], compare_op=ALU.is_ge, fill=0.0,
             base=255, channel_multiplier=256), reads=[KA[j]], writes=[KA[j]])
        P.op("pool", lambda e: e.memset(Xpad[j][:], 0.0), writes=[Xpad[j]])
    ip = 0
    for h in range(16):
        j = h % NB
        hh = h % 4
        jw = (h // 4) % 2
        slope = float(2.0 ** (-(h + 1) / 2.0))
        if hh == 0:
            c0w = (h // 4) * 256
            for (wt, cb) in ((wq4[jw], 0), (wk4[jw], 1024), (wv4[jw], 2048)):
                for k4 in range(4):
                    P.dma("pool", lambda e: e.dma_start(out=wt[:, k4 * 4:(k4 + 1) * 4, :],
                          in_=w_in[k4 * 512:(k4 + 1) * 512, cb + c0w: cb + c0w + 256].rearrange("(k p) c -> p k c", p=128)), writes=[wt])
        for (wt, dstT) in ((wq4[jw], QA[j]), (wk4[jw], KA[j])):
            for g in range(4):
                pq = psf(C, (0, 1, 2, 3))
                for kc in range(16):
                    P.op("pe", lambda e: e.matmul(pq[0:64, :], lhsT=wt[:, kc, hh * 64:(hh + 1) * 64], rhs=hT["t"][:, kc, g * 512:(g + 1) * 512],
                         start=(kc == 0), stop=(kc == 15)), reads=[wt] + allh[g * 4:(g + 1) * 4], writes=[pq])
                P.op("act", lambda e: e.copy(out=dstT[0:64, g * 512:(g + 1) * 512], in_=pq[0:64, :]), reads=[pq], writes=[dstT])
        for half in range(2):
            pv = psf(C, (0, 1, 2, 3))
            for q in range(8):
                it = half * 8 + q
                for kc in range(16):
                    P.op("pe", lambda e: e.matmul(pv[:, q * 64:(q + 1) * 64], lhsT=hT["t"][:, kc, it * 128:(it + 1) * 128],
                         rhs=wv4[jw][:, kc, hh * 64:(hh + 1) * 64], start=(kc == 0), stop=(kc == 15)), reads=[wv4[jw], allh[it]], writes=[pv])
            P.op("dve", lambda e: e.tensor_copy(out=VA[j][:, half * 8:(half + 1) * 8, :], in_=pv[:].rearrange("p (q d) -> p q d", q=8)),
                 reads=[pv], writes=[VA[j]])
        P.op("dve", lambda e: e.tensor_reduce(out=km[j][:], in_=KA[j][0:64, :].rearrange("p (n k) -> p n k", k=256), axis=AX.X, op=ALU.add),
             reads=[KA[j]], writes=[km[j]])
        P.op("dve", lambda e: e.tensor_copy(out=kmB[j][:], in_=km[j][:]), reads=[km[j]], writes=[kmB[j]])
        pgt = psf(C, (0, 1, 2, 3))
        for it in range(NT):
            P.op("pe", lambda e: e.matmul(pgt[:, it * 8:(it + 1) * 8], lhsT=QA[j][0:64, it * 128:(it + 1) * 128], rhs=kmB[j][:], start=True, stop=True),
                 reads=[QA[j], kmB[j]], writes=[pgt])
        gm_, rk = gm[j], rank[j]
        gflat = lambda t: t[:].rearrange("p a b -> p (a b)")
        P.op("dve", lambda e: e.tensor_tensor(out=gflat(gm_), in0=pgt[:, 0:128], in1=gflat(negfill), op=ALU.add), reads=[pgt, negfill], writes=[gm_])
        P.op("dve", lambda e: e.tensor_tensor(out=cmp[0][:], in0=gm_[:].unsqueeze(2).to_broadcast([128, 16, 8, 8]),
             in1=gm_[:].unsqueeze(3).to_broadcast([128, 16, 8, 8]), op=ALU.is_gt), reads=[gm_], writes=[cmp[0]])
        P.op("dve", lambda e: e.tensor_reduce(out=rk[:], in_=cmp[0][:], axis=AX.X, op=ALU.add), reads=[cmp[0]], writes=[rk])
        P.op("dve", lambda e: e.tensor_scalar(out=rk[:], in0=rk[:], scalar1=3.0, scalar2=None, op0=ALU.is_lt), reads=[rk], writes=[rk])
        P.op("dve", lambda e: e.tensor_tensor(out=rk[:], in0=rk[:], in1=notpast[:], op=ALU.max), reads=[rk, notpast], writes=[rk])
        P.op("dve", lambda e: e.tensor_scalar(out=rk[:], in0=rk[:], scalar1=-1.0, scalar2=BIGNEG, op0=ALU.add, op1=ALU.mult), reads=[rk], writes=[rk])
        P.op("dve", lambda e: e.tensor_scalar(out=cqh[j][:], in0=cqBase[:], scalar1=8.0 * slope, scalar2=None, op0=ALU.mult), reads=[cqBase], writes=[cqh[j]])
        P.op("dve", lambda e: e.tensor_tensor(out=Xpad[j][:, :, 64:72].rearrange("p (g r) n -> p g r n", r=4),
             in0=rk[:].rearrange("p (g r) n -> p g r n", r=4),
             in1=cqh[j][:].unsqueeze(1).unsqueeze(3).to_broadcast([128, 4, 4, 8]), op=ALU.add), reads=[rk, cqh[j]], writes=[Xpad[j]])
        for half in range(2):
            pb = psb(C)
            for q in range(8):
                it = half * 8 + q
                P.op("pe", lambda e: e.transpose(out=pb[:, q * 128:(q + 1) * 128], in_=Xpad[j][:, it, :], identity=C.identB[:]),
                     reads=[Xpad[j], C.identB], writes=[pb])
            P.op("act", lambda e: e.copy(out=QA[j][64:72, half * 1024:(half + 1) * 1024], in_=pb[64:72, 0:1024]), reads=[pb], writes=[QA[j]])
        P.op("dve", lambda e: e.tensor_scalar(out=bk[j][:], in0=biasBase[:], scalar1=slope, scalar2=None, op0=ALU.mult), reads=[biasBase], writes=[bk[j]])
        for g in range(4):
            po, pd = C.psF[4], C.psF[5]
            nkt = 4 * g + 4
            for kt in range(nkt):
                i = kt - 4 * g
                q0 = g * 512 + (128 * i if i > 0 else 0)
                n = (g + 1) * 512 - q0
                c0 = q0 - g * 512
                ps_ = psf(C, (0, 1, 2))
                P.op("pe", lambda e: e.matmul(ps_[:, 0:n], lhsT=KA[j][0:72, kt * 128:(kt + 1) * 128], rhs=QA[j][0:72, q0:q0 + n], start=True, stop=(i < 0)),
                     reads=[KA[j], QA[j]], writes=[ps_])
                p_ = pT[ip % 4]; ip += 1
                if i >= 0:
                    P.op("pe", lambda e: e.matmul(ps_[:, 0:128], lhsT=C.identB[:], rhs=triN[:], start=False, stop=True), reads=[C.identB, triN], writes=[ps_])
                P.op("act", lambda e: e.activation(out=p_[:, 0:n], in_=ps_[:, 0:n], func=AF.Exp, scale=MOBA_SCALE, bias=bk[j][:, i + 12:i + 13]),
                     reads=[ps_, bk[j]], writes=[p_])
                P.op("pe", lambda e: e.matmul(po[0:64, c0:512], lhsT=VA[j][:, kt, :], rhs=p_[:, 0:n], start=(kt == 0), stop=(kt == nkt - 1)),
                     reads=[VA[j], p_], writes=[po])
                P.op("pe", lambda e: e.matmul(pd[0:1, c0:512], lhsT=onesK[:], rhs=p_[:, 0:n], start=(kt == 0), stop=(kt == nkt - 1)),
                     reads=[onesK, p_], writes=[pd])
            ob, rd = osb[g % 2], rden[g % 2]
            P.op("act", lambda e: e.copy(out=ob[:], in_=po[0:64, :]), reads=[po], writes=[ob])
            P.op("dve", lambda e: e.reciprocal(out=rd[:], in_=pd[0:1, :]), reads=[pd], writes=[rd])
            pbc = C.psF[3]
            P.op("pe", lambda e: e.matmul(pbc[0:64, :], lhsT=onesF[0:1, 0:64], rhs=rd[:], start=True, stop=True), reads=[onesF, rd], writes=[pbc])
            P.op("dve", lambda e: e.tensor_tensor(out=aS[j][:, g * 512:(g + 1) * 512], in0=ob[:], in1=pbc[0:64, :], op=ALU.mult),
                 reads=[ob, pbc], writes=[aS[j]])
        P.dma("sp", lambda e: e.dma_start(out=mixT_hbm[h * 64:(h + 1) * 64, :], in_=aS[j][:]), reads=[aS[j]], writes=[C.mixT_b])


RW_IN = 3360
RW_L = 64
RW_NC = S // RW_L


def load_cols(C, vec_row, n, name, eng_q="sp"):
    P = C.P
    raw = P.sb([n, 128], F32, name + "_raw")
    out = P.sb([128, n], F32, name)
    P.dma(eng_q, lambda e: e.dma_start(out=raw[:], in_=vec_row.rearrange("o (c p) -> (o c) p", p=128)), writes=[raw])
    pt = psf(C, (0, 1, 2, 3))
    P.op("pe", lambda e: e.transpose(out=pt[:, 0:n], in_=raw[:], identity=C.identF[0:n, 0:n]), reads=[raw, C.identF], writes=[pt])
    P.op("act", lambda e: e.copy(out=out[:], in_=pt[:, 0:n]), reads=[pt], writes=[out])
    return out


def stage_rwkv_proj(C, *, w_in, mu_row, pp_hbm):
    P = C.P
    P.barrier()
    P.reset()
    hT = C.hT
    mu26 = load_cols(C, mu_row[:, 0:3328], 26, "mu26")
    mul = load_cols(C, mu_row[:, 3232:3360], 1, "mul")
    om26 = P.sb([128, 26], F32, "om26")
    oml = P.sb([128, 1], F32, "oml")
    P.op("dve", lambda e: e.tensor_scalar(out=om26[:], in0=mu26[:], scalar1=-1.0, scalar2=1.0, op0=ALU.mult, op1=ALU.add), reads=[mu26], writes=[om26])
    P.op("dve", lambda e: e.tensor_scalar(out=oml[:], in0=mul[:], scalar1=-1.0, scalar2=1.0, op0=ALU.mult, op1=ALU.add), reads=[mul], writes=[oml])
    wt = [P.sb([128, 16, 128], BF16, "rwt") for _ in range(2)]
    pc = [P.sb([128, S + 1], F32, "rpc") for _ in range(2)]
    tmp = [P.sb([128, S], F32, "rtmp") for _ in range(2)]
    for b in range(2):
        P.op("pool", lambda e: e.memset(pc[b][:, 0:1], 0.0), writes=[pc[b]])
    for c in range(27):
        j = c % 2
        r0 = c * 128 if c < 26 else RW_IN - 128
        col0 = 3072 + r0
        for k4 in range(4):
            P.dma("pool", lambda e: e.dma_start(out=wt[j][:, k4 * 4:(k4 + 1) * 4, :],
                  in_=w_in[k4 * 512:(k4 + 1) * 512, col0:col0 + 128].rearrange("(k p) c -> p k c", p=128)), writes=[wt[j]])
        for g in range(4):
            pq = psf(C, (0, 1, 2, 3))
            for kc in range(16):
                P.op("pe", lambda e: e.matmul(pq[:], lhsT=wt[j][:, kc, :], rhs=hT["t"][:, kc, g * 512:(g + 1) * 512],
                     start=(kc == 0), stop=(kc == 15)), reads=[wt[j]] + hT["b"][g * 4:(g + 1) * 4], writes=[pq])
            P.op("act", lambda e: e.copy(out=pc[j][:, 1 + g * 512:1 + (g + 1) * 512], in_=pq[:]), reads=[pq], writes=[pc[j]])
        mu_ap = mu26[:, c:c + 1] if c < 26 else mul[:, 0:1]
        om_ap = om26[:, c:c + 1] if c < 26 else oml[:, 0:1]
        P.op("dve", lambda e: e.tensor_scalar(out=tmp[j][:], in0=pc[j][:, 0:S], scalar1=mu_ap, scalar2=None, op0=ALU.mult),
             reads=[pc[j], mu26, mul], writes=[tmp[j]])
        P.op("dve", lambda e: e.scalar_tensor_tensor(out=tmp[j][:], in0=pc[j][:, 1:S + 1], scalar=om_ap, in1=tmp[j][:],
             op0=ALU.mult, op1=ALU.add), reads=[pc[j], om26, oml, tmp[j]], writes=[tmp[j]])
        P.dma("sp", lambda e: e.dma_start(out=pp_hbm[r0:r0 + 128, :], in_=tmp[j][:]), reads=[tmp[j]], writes=[C.pp_b])


RW_C0 = 0.6065306597126334


def stage_rwkv_main(C, *, pp_hbm, prm, vfirst_hbm, first, mixT_hbm, dbg=None):
    nc, P = C.nc, C.P
    import os as _os
    STOP = int(_os.environ.get("RW_STOP", "99"))
    NPAIR = int(_os.environ.get("RW_NPAIR", "8"))
    P.barrier()
    P.reset()
    P.sb_off = C.hT_off
    L, NC = RW_L, RW_NC
    twd = P.sb([64, S], BF16, "twd"); adb = P.sb([64, S], BF16, "adb")
    sgd0 = P.sb([128, S], BF16, "sgd0"); sgd1_ = P.sb([64, S], BF16, "sgd1"); sgd1 = sgd1_
    vv1b = P.sb([64, S], BF16, "vv1b")
    w2b = P.sb([64, 1024], BF16, "w2b"); a2b = P.sb([64, 1024], BF16, "a2b")
    g2b0 = P.sb([128, 1024], BF16, "g2b0"); g2b1 = P.sb([64, 1024], BF16, "g2b1")
    P.dma("pool", lambda e: e.dma_start(out=w2b[:], in_=prm["w2"]), writes=[w2b])
    P.dma("pool", lambda e: e.dma_start(out=a2b[:], in_=prm["a2"]), writes=[a2b])
    P.dma("pool", lambda e: e.dma_start(out=g2b0[:], in_=prm["g2"][0:128, :]), writes=[g2b0])
    P.dma("pool", lambda e: e.dma_start(out=g2b1[0:32, :], in_=prm["g2"][128:160, :]), writes=[g2b1])
    if STOP <= -2:
        return
    cols = {nm: load_cols(C, prm[nm], 8, "c_" + nm) for nm in (("w0", "a0", "k_k", "k_a", "r_k") + (() if first else ("v0",)))}
    lnwB = P.sb([64, 128], F32, "lnwB"); lnbB = P.sb([64, 128], F32, "lnbB")
    if STOP <= -1:
        return
    mSU = P.sb([64, 64], F32, "mSU"); mSL = P.sb([64, 64], F32, "mSL"); mIU = P.sb([64, 64], F32, "mIU")
    rmask = P.sb([128, S], BF16, "rmask")
    headsel = P.sb([128, 2], F32, "headsel")
    bones = P.sb([128, 128], F32, "bones")
    for t in (mSU, mSL, mIU, rmask, headsel, bones):
        P.op("pool", lambda e: e.memset(t[:], 1.0), writes=[t])
    P.op("pool", lambda e: e.affine_select(out=mSU[:], in_=mSU[:], pattern=[[1, 64]], compare_op=ALU.is_gt, fill=0.0, base=0, channel_multiplier=-1), reads=[mSU], writes=[mSU])
    P.op("pool", lambda e: e.affine_select(out=mSL[:], in_=mSL[:], pattern=[[-1, 64]], compare_op=ALU.is_gt, fill=0.0, base=0, channel_multiplier=1), reads=[mSL], writes=[mSL])
    P.op("pool", lambda e: e.affine_select(out=mIU[:], in_=mIU[:], pattern=[[1, 64]], compare_op=ALU.is_ge, fill=0.0, base=0, channel_multiplier=-1), reads=[mIU], writes=[mIU])
    P.op("pool", lambda e: e.memset(rmask[:].rearrange("p (c l) -> p c l", l=L)[:, :, 0:1], 0.0), reads=[rmask], writes=[rmask])
    P.op("pool", lambda e: e.memset(headsel[0:64, 1:2], 0.0), reads=[headsel], writes=[headsel])
    P.op("pool", lambda e: e.memset(headsel[64:128, 0:1], 0.0), reads=[headsel], writes=[headsel])
    P.op("pool", lambda e: e.memset(bones[0:64, 64:128], 0.0), reads=[bones], writes=[bones])
    P.op("pool", lambda e: e.memset(bones[64:128, 0:64], 0.0), reads=[bones], writes=[bones])
    epsg = P.sb([64, 1], F32, "epsg")
    P.op("pool", lambda e: e.memset(epsg[:], 64e-5), writes=[epsg])
    if STOP <= 0:
        return
    big = lambda nm: P.sb([128, S], F32, nm)
    offR = (P.sb_off + 63) // 64 * 64
    R = big("R")
    offK = (P.sb_off + 63) // 64 * 64
    K_, V = big("K"), big("V")
    offA = (P.sb_off + 63) // 64 * 64
    A, LW = big("A"), big("LW")
    offCUM = (P.sb_off + 63) // 64 * 64
    CUM, G = big("CUM"), big("G")
    offKK = (P.sb_off + 63) // 64 * 64
    KK, AT, BT, KT, BG, KG, PROD = big("KK"), big("AT"), big("BT"), big("KT"), big("BG"), big("KG"), big("PROD")
    offE = (P.sb_off + 63) // 64 * 64
    E1, E2 = big("E1"), big("E2")
    GL = P.sb([128, NC], F32, "GL")
    VtokAll = P.sb_at(offA, [64, NC, 128], F32, "VtokAll")
    Ytok = P.sb_at(offE, [64, NC, 128], F32, "Ytok")
    ST = P.sb([128, 64], F32, "ST")
    STb = P.sb([128, 64], BF16, "STb")
    def carve(base_tl_off, items):
        out, off = [], base_tl_off
        for (shape, dt, nm) in items:
            esz = 2 if dt == BF16 else 4
            out.append(P.sb_at(off, shape, dt, nm))
            off += int(np.prod(shape[1:])) * esz
            off = (off + 63) // 64 * 64
        assert off - base_tl_off <= S * 4
        return out
    SETS = []
    kcarve = carve(offK, [([64, 8, 64], BF16, "PQ")] * 8)
    kkcarve = carve(offKK, [([64, 8, 64], F32, "Mi")] * 2 + [([64, 3, 128], BF16, "Tok")] * 4)
    for si in range(2):
        st_ = {}
        st_["Xm"] = {nm: [P.sb([128, 4 * RW_L], BF16, "Xm" + nm) for _ in range(2)] for nm in ("AT", "BT", "KT", "R")}
        st_["Yb"] = {nm: P.sb([128, 4 * RW_L], BF16, "Yb" + nm) for nm in ("AT", "BT", "R")}
        st_["Msb"] = {nm: P.sb([64, 8, 64], BF16, "M" + nm) for nm in ("AK", "RB", "RK")}
        st_["MiB"] = P.sb([64, 8, 64], BF16, "MiB")
        st_["Pb"] = [kcarve[si * 4 + 0], kcarve[si * 4 + 1]]
        st_["Qb"] = [kcarve[si * 4 + 2], kcarve[si * 4 + 3]]
        st_["Mi"] = kkcarve[si]
        st_["Tok"] = kkcarve[2:6] if si == 0 else [P.sb([64, 3, 128], BF16, "Tok2") for _ in range(4)]
        SETS.append(st_)
    BO, Wsb, Usb, BON, stt = carve(offCUM, [([128, S], BF16, "BO"), ([64, 128], BF16, "Wsb"), ([64, 128], BF16, "Usb"),
                                            ([64, NC, 2], F32, "BON"), ([64, 4, 64], F32, "stt")])
    def ldrow(dst, row0, n):
        P.dma("sp", lambda e: e.dma_start(out=dst, in_=pp_hbm[row0:row0 + n, :]), reads=[C.pp_b], writes=[C.sc_b])
    NP2 = int(_os.environ.get("RW_P2", "4"))
    if NP2 >= 1:
        ldrow(E1[0:64, :], 3072, 64)
        P.op("act", lambda e: e.activation(out=twd[:], in_=E1[0:64, :], func=AF.Tanh), reads=[C.sc_b], writes=[twd, C.sc_b])
    if NP2 >= 2:
        ldrow(E2[0:64, :], 3136, 64)
        P.op("act", lambda e: e.copy(out=adb[:], in_=E2[0:64, :]), reads=[C.sc_b], writes=[adb, C.sc_b])
    if NP2 >= 3:
        ldrow(AT[:, :], 3200, 128)
        P.op("act", lambda e: e.activation(out=sgd0[:], in_=AT[:, :], func=AF.Sigmoid), reads=[C.sc_b], writes=[sgd0, C.sc_b])
    if NP2 >= 4:
        ldrow(BT[0:32, :], 3328, 32)
        P.op("act", lambda e: e.activation(out=sgd1[0:32, :], in_=BT[0:32, :], func=AF.Sigmoid), reads=[C.sc_b], writes=[sgd1, C.sc_b])
    if not first:
        v1b = P.sb([128, 8, 32], BF16, "v1b"); v2b = P.sb([64, 1024], BF16, "v2b")
        P.dma("pool", lambda e: e.dma_start(out=v1b[:], in_=prm["v1"].rearrange("(k p) c -> p k c", p=128)), writes=[v1b])
        P.dma("pool", lambda e: e.dma_start(out=v2b[0:32, :], in_=prm["v2"]), writes=[v2b])
        vb = carve(offR, [([128, S], BF16, "vb")] * 2)
        pvv = [C.psF[i] for i in range(4)]
        for c in range(8):
            P.dma("pool", lambda e: e.dma_start(out=vb[c % 2][:], in_=pp_hbm[2048 + c * 128:2048 + (c + 1) * 128, :]), reads=[C.pp_b], writes=[vb[c % 2]])
            for g in range(4):
                P.op("pe", lambda e: e.matmul(pvv[g][0:32, :], lhsT=v1b[:, c, :], rhs=vb[c % 2][:, g * 512:(g + 1) * 512], start=(c == 0), stop=(c == 7)),
                     reads=[v1b, vb[c % 2]], writes=[pvv[g]])
        for g in range(4):
            P.op("act", lambda e: e.copy(out=vv1b[0:32, g * 512:(g + 1) * 512], in_=pvv[g][0:32, :]), reads=[pvv[g]], writes=[vv1b])
    P.barrier()
    if STOP <= 1:
        return
    PREPK = int(_os.environ.get("RW_PREP", "99"))
    AM = _os.environ.get("RW_AM", "")
    for c in range(NPAIR):
        cs = slice(c * 128, (c + 1) * 128)
        sc = C.sc_b
        def dv(fn, extra_r=(), extra_w=()):
            P.op("dve", fn, reads=[sc] + list(extra_r), writes=[sc] + list(extra_w))
        def ac(fn, extra_r=(), extra_w=()):
            P.op("act", fn, reads=[sc] + list(extra_r), writes=[sc] + list(extra_w))
        P.dma("sp", lambda e: e.dma_start(out=lnwB[:], in_=bcast_rows(prm["ln_w"][:, cs], 64)), reads=[sc], writes=[lnwB, sc])
        P.dma("sp", lambda e: e.dma_start(out=lnbB[:], in_=bcast_rows(prm["ln_b"][:, cs], 64)), reads=[sc], writes=[lnbB, sc])
        for (dst, r0) in ((R, 0), (K_, 1024), (V, 2048)):
            P.dma("sp", lambda e: e.dma_start(out=dst[:], in_=pp_hbm[r0 + c * 128:r0 + (c + 1) * 128, :]), reads=[C.pp_b, sc], writes=[sc])
        for g in range(4):
            gs = slice(g * 512, (g + 1) * 512)
            pz = psf(C, (0, 1, 2, 3))
            P.op("pe", lambda e: e.matmul(pz[:], lhsT=w2b[:, cs], rhs=twd[:, gs], start=True, stop=True), reads=[w2b, twd], writes=[pz])
            ac(lambda e: e.activation(out=LW[:, gs], in_=pz[:], func=AF.Sigmoid, bias=cols["w0"][:, c:c + 1]), extra_r=[pz, cols["w0"]])
            pa = psf(C, (0, 1, 2, 3))
            P.op("pe", lambda e: e.matmul(pa[:], lhsT=a2b[:, cs], rhs=adb[:, gs], start=True, stop=True), reads=[a2b, adb], writes=[pa])
            ac(lambda e: e.activation(out=A[:, gs], in_=pa[:], func=AF.Sigmoid, bias=cols["a0"][:, c:c + 1]), extra_r=[pa, cols["a0"]])
            pg = psf(C, (0, 1, 2, 3))
            P.op("pe", lambda e: e.matmul(pg[:], lhsT=g2b0[:, cs], rhs=sgd0[:, gs], start=True, stop=False), reads=[g2b0, sgd0], writes=[pg])
            P.op("pe", lambda e: e.matmul(pg[:], lhsT=g2b1[0:32, cs], rhs=sgd1[0:32, gs], start=False, stop=True), reads=[g2b1, sgd1], writes=[pg])
            ac(lambda e: e.copy(out=G[:, gs], in_=pg[:]), extra_r=[pg])
            if not first:
                pl = psf(C, (0, 1, 2, 3))
                P.op("pe", lambda e: e.matmul(pl[:], lhsT=v2b[0:32, cs], rhs=vv1b[0:32, gs], start=True, stop=True), reads=[v2b, vv1b], writes=[pl])
                ac(lambda e: e.activation(out=E2[:, gs], in_=pl[:], func=AF.Sigmoid, bias=cols["v0"][:, c:c + 1]), extra_r=[pl, cols["v0"]])
        if PREPK <= 1:
            return
        if not first:
            P.dma("sp", lambda e: e.dma_start(out=E1[:], in_=vfirst_hbm[cs, :]), reads=[sc, C.vf_b], writes=[sc])
            dv(lambda e: e.tensor_tensor(out=E1[:], in0=E1[:], in1=V[:], op=ALU.subtract))
            dv(lambda e: e.tensor_tensor(out=E1[:], in0=E1[:], in1=E2[:], op=ALU.mult))
            dv(lambda e: e.tensor_tensor(out=V[:], in0=V[:], in1=E1[:], op=ALU.add))
        else:
            P.dma("sp", lambda e: e.dma_start(out=vfirst_hbm[cs, :], in_=V[:]), reads=[sc], writes=[C.vf_b])
        if PREPK <= 2:
            return
        dv(lambda e: e.tensor_scalar(out=KK[:], in0=K_[:], scalar1=cols["k_k"][:, c:c + 1], scalar2=None, op0=ALU.mult), extra_r=[cols["k_k"]])
        ac(lambda e: e.activation(out=E1[:], in_=KK[:], func=AF.Square))
        for g in range(4):
            gs = slice(g * 512, (g + 1) * 512)
            pss = psf(C, (0, 1, 2, 3))
            P.op("pe", lambda e: e.matmul(pss[:], lhsT=bones[:], rhs=E1[:, gs], start=True, stop=True), reads=[bones, sc], writes=[pss])
            ac(lambda e: e.activation(out=E2[:, gs], in_=pss[:], func=AF.Sqrt), extra_r=[pss])
        if PREPK <= 3:
            return
        dv(lambda e: e.tensor_scalar(out=E2[:], in0=E2[:], scalar1=1e-12, scalar2=None, op0=ALU.max))
        dv(lambda e: e.reciprocal(out=E2[:], in_=E2[:]))
        dv(lambda e: e.tensor_tensor(out=KK[:], in0=KK[:], in1=E2[:], op=ALU.mult))
        if PREPK <= 4:
            return
        dv(lambda e: e.tensor_scalar(out=E1[:], in0=A[:], scalar1=1.0, scalar2=cols["k_a"][:, c:c + 1], op0=ALU.subtract, op1=ALU.mult), extra_r=[cols["k_a"]])
        dv(lambda e: e.scalar_tensor_tensor(out=K_[:], in0=E1[:], scalar=1.0, in1=K_[:], op0=ALU.add, op1=ALU.mult))
        dv(lambda e: e.scalar_tensor_tensor(out=PROD[:], in0=R[:], scalar=cols["r_k"][:, c:c + 1], in1=K_[:], op0=ALU.mult, op1=ALU.mult), extra_r=[cols["r_k"]])
        if PREPK <= 5:
            return
        dv(lambda e: e.tensor_tensor_scan(out=CUM[:], data0=rmask[:], data1=LW[:], initial=0.0, op0=ALU.mult, op1=ALU.add), extra_r=[rmask])
        if PREPK <= 6:
            return
        ac(lambda e: e.activation(out=E1[:], in_=CUM[:], func=AF.Exp, scale=-RW_C0))
        dv(lambda e: e.tensor_tensor(out=R[:], in0=R[:], in1=E1[:], op=ALU.mult))
        dv(lambda e: e.tensor_tensor(out=E2[:], in0=CUM[:], in1=LW[:], op=ALU.subtract))
        ac(lambda e: e.activation(out=E2[:], in_=E2[:], func=AF.Exp, scale=-RW_C0))
        dv(lambda e: e.scalar_tensor_tensor(out=AT[:], in0=KK[:], scalar=-1.0, in1=E2[:], op0=ALU.mult, op1=ALU.mult))
        dv(lambda e: e.tensor_tensor(out=BG[:], in0=KK[:], in1=A[:], op=ALU.mult))
        ac(lambda e: e.activation(out=E1[:], in_=CUM[:], func=AF.Exp, scale=RW_C0))
        dv(lambda e: e.tensor_tensor(out=BT[:], in0=BG[:], in1=E1[:], op=ALU.mult))
        dv(lambda e: e.tensor_tensor(out=KT[:], in0=K_[:], in1=E1[:], op=ALU.mult))
        if PREPK <= 7:
            return
        c3 = lambda t: t[:].rearrange("p (c l) -> p c l", l=L)
        dv(lambda e: e.tensor_tensor(out=c3(E2), in0=c3(CUM)[:, :, L - 1:L].to_broadcast([128, NC, L]), in1=c3(CUM), op=ALU.subtract))
        ac(lambda e: e.activation(out=E2[:], in_=E2[:], func=AF.Exp, scale=-RW_C0))
        dv(lambda e: e.tensor_tensor(out=BG[:], in0=BG[:], in1=E2[:], op=ALU.mult))
        dv(lambda e: e.tensor_tensor(out=KG[:], in0=K_[:], in1=E2[:], op=ALU.mult))
        if PREPK <= 8:
            return
        ac(lambda e: e.activation(out=GL[:].unsqueeze(2), in_=c3(CUM)[:, :, L - 1:L], func=AF.Exp, scale=-RW_C0))
        if STOP <= 2:
            return
        P.barrier()
        NG = NC // 4

        def pre_part(grp, st_, part):
            Xm, Yb, Msb, MiB, Pb, Qb, Mi, Tok = (st_[k] for k in ("Xm", "Yb", "Msb", "MiB", "Pb", "Qb", "Mi", "Tok"))
            g0 = grp * 4 * L
            if part == 0:
                for nm, X in (("AT", AT), ("BT", BT), ("KT", KT), ("R", R)):
                    for hd in range(2):
                        P.op("dve" if hd == 0 else "pool", lambda e: e.tensor_scalar(out=Xm[nm][hd][:], in0=X[:, g0:g0 + 4 * L],
                             scalar1=headsel[:, hd:hd + 1], scalar2=None, op0=ALU.mult), reads=[sc, headsel], writes=[Xm[nm][hd]])
                for nm, X in (("AT", AT), ("BT", BT), ("R", R)):
                    P.op("act", lambda e: e.copy(out=Yb[nm][:], in_=X[:, g0:g0 + 4 * L]), reads=[sc], writes=[Yb[nm]])

                def amat(xn, yn, mask, dst):
                    pm = psf(C, (0, 1, 2))
                    for q in range(4):
                        for hd in range(2):
                            m = q * 2 + hd
                            P.op("pe", lambda e: e.matmul(pm[0:64, m * 64:(m + 1) * 64], lhsT=Xm[xn][hd][:, q * L:(q + 1) * L],
                                 rhs=Yb[yn][:, q * L:(q + 1) * L], start=True, stop=True), reads=[Xm[xn][hd], Yb[yn]], writes=[pm])
                    P.op("dve", lambda e: e.tensor_tensor(out=dst[:], in0=pm[0:64, :].rearrange("p (m t) -> p m t", m=8),
                         in1=mask[:].unsqueeze(1).to_broadcast([64, 8, 64]), op=ALU.mult), reads=[pm, mask], writes=[dst])
                amat("BT", "AT", mSU, Pb[0])
                amat("AT", "BT", mSL, Qb[0])
                amat("KT", "AT", mSU, Msb["AK"])
                amat("BT", "R", mIU, Msb["RB"])
                amat("KT", "R", mIU, Msb["RK"])
                P.op("dve", lambda e: e.tensor_tensor(out=Mi[:], in0=Pb[0][:], in1=C.identF[0:64, 0:64].unsqueeze(1).to_broadcast([64, 8, 64]), op=ALU.add),
                     reads=[Pb[0], C.identF], writes=[Mi])
                P.op("act", lambda e: e.copy(out=MiB[:], in_=Mi[:]), reads=[Mi], writes=[MiB])
            elif 1 <= part <= 5:
                cur = (part - 1) % 2
                nx = 1 - cur
                pP, pQ = psf(C, (0, 1, 2)), psf(C, (0, 1, 2))
                for m in range(8):
                    P.op("pe", lambda e: e.matmul(pP[0:64, m * 64:(m + 1) * 64], lhsT=Qb[cur][:, m, :], rhs=Pb[cur][:, m, :], start=True, stop=True),
                         reads=[Qb[cur], Pb[cur]], writes=[pP])
                for m in range(8):
                    P.op("pe", lambda e: e.matmul(pQ[0:64, m * 64:(m + 1) * 64], lhsT=Pb[cur][:, m, :], rhs=Qb[cur][:, m, :], start=True, stop=True),
                         reads=[Qb[cur], Pb[cur]], writes=[pQ])
                P.op("act", lambda e: e.copy(out=Pb[nx][:].rearrange("p m t -> p (m t)"), in_=pP[0:64, :]), reads=[pP], writes=[Pb[nx]])
                P.op("dve", lambda e: e.tensor_copy(out=Qb[nx][:].rearrange("p m t -> p (m t)"), in_=pQ[0:64, :]), reads=[pQ], writes=[Qb[nx]])
                pM = psf(C, (0, 1, 2))
                for m in range(8):
                    P.op("pe", lambda e: e.matmul(pM[0:64, m * 64:(m + 1) * 64], lhsT=Qb[nx][:, m, :], rhs=MiB[:, m, :], start=True, stop=True),
                         reads=[Qb[nx], MiB], writes=[pM])
                P.op("dve", lambda e: e.tensor_tensor(out=Mi[:].rearrange("p m t -> p (m t)"), in0=Mi[:].rearrange("p m t -> p (m t)"), in1=pM[0:64, :], op=ALU.add),
                     reads=[pM, Mi], writes=[Mi])
                P.op("act", lambda e: e.copy(out=MiB[:], in_=Mi[:]), reads=[Mi], writes=[MiB])
            else:
                for q in range(4):
                    ci = grp * 4 + q
                    t0 = ci * L
                    pt = C.psF[3]
                    for i3, X in enumerate((BG, KG, V)):
                        P.op("pe", lambda e: e.transpose(out=pt[0:64, i3 * 128:(i3 + 1) * 128], in_=X[:, t0:t0 + L], identity=C.identF[:]),
                             reads=[sc, C.identF], writes=[pt])
                    P.op("act", lambda e: e.copy(out=Tok[q][:].rearrange("p a b -> p (a b)"), in_=pt[0:64, 0:384]), reads=[pt], writes=[Tok[q]])
                    P.op("dve", lambda e: e.tensor_copy(out=VtokAll[:, ci, :], in_=pt[0:64, 256:384]), reads=[pt, Tok[q]], writes=[VtokAll])

        def seq_chunk(grp, st_, q):
            Xm, Msb, MiB, Tok = (st_[k] for k in ("Xm", "Msb", "MiB", "Tok"))
            ci = grp * 4 + q
            pw, py = C.psF[4], C.psF[5]
            if ci == 0:
                P.op("pool", lambda e: e.memset(ST[:], 0.0), writes=[ST])
                P.op("pool", lambda e: e.memset(STb[:], 0.0), writes=[STb])
            for hd in range(2):
                m = q * 2 + hd
                hs = slice(hd * 64, (hd + 1) * 64)
                P.op("pe", lambda e: e.matmul(pw[0:64, hs], lhsT=Xm["AT"][hd][:, q * L:(q + 1) * L], rhs=STb[:, :], start=True, stop=False),
                     reads=[Xm["AT"][hd], STb], writes=[pw])
                P.op("pe", lambda e: e.matmul(pw[0:64, hs], lhsT=Msb["AK"][:, m, :], rhs=Tok[q][:, 2, hs], start=False, stop=True),
                     reads=[Msb["AK"], Tok[q]], writes=[pw])
            P.op("act", lambda e: e.copy(out=Wsb[:], in_=pw[0:64, 0:128]), reads=[pw], writes=[Wsb])
            for hd in range(2):
                m = q * 2 + hd
                hs = slice(hd * 64, (hd + 1) * 64)
                P.op("pe", lambda e: e.matmul(pw[0:64, 128 + hd * 64:128 + (hd + 1) * 64], lhsT=MiB[:, m, :], rhs=Wsb[:, hs], start=True, stop=True),
                     reads=[MiB, Wsb], writes=[pw])
            P.op("dve", lambda e: e.tensor_copy(out=Usb[:], in_=pw[0:64, 128:256]), reads=[pw], writes=[Usb])
            for hd in range(2):
                m = q * 2 + hd
                hs = slice(hd * 64, (hd + 1) * 64)
                P.op("pe", lambda e: e.matmul(py[0:64, hs], lhsT=Xm["R"][hd][:, q * L:(q + 1) * L], rhs=STb[:, :], start=True, stop=False),
                     reads=[Xm["R"][hd], STb], writes=[py])
                P.op("pe", lambda e: e.matmul(py[0:64, hs], lhsT=Msb["RB"][:, m, :], rhs=Usb[:, hs], start=False, stop=False), reads=[Msb["RB"], Usb], writes=[py])
                P.op("pe", lambda e: e.matmul(py[0:64, hs], lhsT=Msb["RK"][:, m, :], rhs=Tok[q][:, 2, hs], start=False, stop=True), reads=[Msb["RK"], Tok[q]], writes=[py])
            P.op("act", lambda e: e.copy(out=Ytok[:, ci, :], in_=py[0:64, 0:128]), reads=[py], writes=[Ytok])
            for hd in range(2):
                hs = slice(hd * 64, (hd + 1) * 64)
                oc = slice(128 + hd * 64, 128 + (hd + 1) * 64)
                P.op("pe", lambda e: e.matmul(py[:, oc], lhsT=Tok[q][:, 0, :], rhs=Usb[:, hs], start=True, stop=False), reads=[Tok[q], Usb], writes=[py])
                P.op("pe", lambda e: e.matmul(py[:, oc], lhsT=Tok[q][:, 1, :], rhs=Tok[q][:, 2, hs], start=False, stop=True), reads=[Tok[q]], writes=[py])
            for hd in range(2):
                hs = slice(hd * 64, (hd + 1) * 64)
                oc = slice(128 + hd * 64, 128 + (hd + 1) * 64)
                P.op("dve", lambda e: e.scalar_tensor_tensor(out=ST[hs, :], in0=ST[hs, :], scalar=GL[hs, ci:ci + 1], in1=py[hs, oc], op0=ALU.mult, op1=ALU.add),
                     reads=[py, ST, sc], writes=[ST])
            P.op("act", lambda e: e.copy(out=STb[:], in_=ST[:]), reads=[ST], writes=[STb])

        for part in range(7):
            pre_part(0, SETS[0], part)
        for grp in range(NG):
            cur_s, nxt_s = SETS[grp % 2], SETS[(grp + 1) % 2]
            has_n = grp + 1 < NG
            order = [("p", 0), ("s", 0), ("p", 1), ("p", 2), ("s", 1), ("p", 3), ("s", 2), ("p", 4), ("p", 5), ("s", 3), ("p", 6)]
            _ord = _os.environ.get("RW_ORD", "s0,p0,s1,p1,p2,s2,p3,p4,s3,p5,p6")
            order = [(t[0], int(t[1:])) for t in _ord.split(",")]
            for kind, idx in order:
                if kind == "s":
                    seq_chunk(grp, cur_s, idx)
                elif has_n:
                    pre_part(grp + 1, nxt_s, idx)
        P.barrier()
        if STOP <= 6:
            return
        pbn = psf(C, (0, 1, 2))
        for ci in range(NC):
            P.op("pe", lambda e: e.matmul(pbn[0:64, ci * 2:(ci + 1) * 2], lhsT=PROD[:, ci * L:(ci + 1) * L], rhs=headsel[:], start=True, stop=True),
                 reads=[sc, headsel], writes=[pbn])
        dv(lambda e: e.tensor_copy(out=BON[:].rearrange("p c h -> p (c h)"), in_=pbn[0:64, 0:2 * NC]), extra_r=[pbn])
        y4 = Ytok[:].rearrange("p c (h i) -> p (c h) i", h=2)
        v4 = VtokAll[:].rearrange("p c (h i) -> p (c h) i", h=2)
        dv(lambda e: e.tensor_reduce(out=stt[:, 0, :], in_=y4, axis=AX.X, op=ALU.add))
        for hf in range(2):
            ysl = Ytok[:, hf * 16:(hf + 1) * 16, :].rearrange("p c (h i) -> p (c h) i", h=2)
            sq = AT[0:64, :].rearrange("p (m i) -> p m i", i=64)
            dv(lambda e: e.tensor_tensor(out=sq, in0=ysl, in1=ysl, op=ALU.mult))
            dv(lambda e: e.tensor_reduce(out=stt[:, 1, hf * 32:(hf + 1) * 32], in_=sq, axis=AX.X, op=ALU.add))
        dv(lambda e: e.tensor_scalar(out=stt[:, 0, :], in0=stt[:, 0, :], scalar1=1.0 / 64.0, scalar2=None, op0=ALU.mult))
        dv(lambda e: e.tensor_tensor(out=stt[:, 2, :], in0=stt[:, 0, :], in1=stt[:, 0, :], op=ALU.mult))
        dv(lambda e: e.scalar_tensor_tensor(out=stt[:, 1, :], in0=stt[:, 1, :], scalar=1.0 / 64.0, in1=stt[:, 2, :], op0=ALU.mult, op1=ALU.subtract))
        ac(lambda e: e.activation(out=stt[:, 1, :], in_=stt[:, 1, :], func=AF.Sqrt, bias=epsg[:, 0:1]), extra_r=[epsg])
        dv(lambda e: e.reciprocal(out=stt[:, 1, :], in_=stt[:, 1, :]))
        dv(lambda e: e.tensor_tensor(out=y4, in0=y4, in1=stt[:, 0, :].unsqueeze(2).to_broadcast([64, 64, 64]), op=ALU.subtract))
        dv(lambda e: e.tensor_tensor(out=y4, in0=y4, in1=stt[:, 1, :].unsqueeze(2).to_broadcast([64, 64, 64]), op=ALU.mult))
        dv(lambda e: e.tensor_tensor(out=Ytok[:], in0=Ytok[:], in1=lnwB[:, :].unsqueeze(1).to_broadcast([64, NC, 128]), op=ALU.mult), extra_r=[lnwB])
        dv(lambda e: e.tensor_tensor(out=Ytok[:], in0=Ytok[:], in1=lnbB[:, :].unsqueeze(1).to_broadcast([64, NC, 128]), op=ALU.add), extra_r=[lnbB])
        dv(lambda e: e.tensor_tensor(out=v4, in0=v4, in1=BON[:].rearrange("p c h -> p (c h)").unsqueeze(2).to_broadcast([64, 64, 64]), op=ALU.mult))
        dv(lambda e: e.tensor_tensor(out=Ytok[:], in0=Ytok[:], in1=VtokAll[:], op=ALU.add))
        for g8 in range(NC // 8):
            pt = psf(C, (0, 1, 2))
            for q in range(8):
                ci = g8 * 8 + q
                P.op("pe", lambda e: e.transpose(out=pt[:, q * 64:(q + 1) * 64], in_=Ytok[:, ci, :], identity=C.identF[0:64, 0:64]),
                     reads=[sc, C.identF], writes=[pt])
            dv(lambda e: e.tensor_tensor(out=BO[:, g8 * 512:(g8 + 1) * 512], in0=pt[:], in1=G[:, g8 * 512:(g8 + 1) * 512], op=ALU.mult), extra_r=[pt])
        P.dma("sp", lambda e: e.dma_start(out=mixT_hbm[1024 + c * 128:1024 + (c + 1) * 128, :], in_=BO[:]), reads=[sc], writes=[C.mixT_b, sc])
    P.barrier()


def stage_outproj_hbm(C, *, mixT_hbm, w_out, mix_hbm):
    P = C.P
    P.barrier()
    P.reset()
    P.sb_off = C.hT_off
    xg = [P.sb([128, 16, 512], BF16, "xg") for _ in range(2)]
    wb = [P.sb([128, 16, 512], BF16, "wob") for _ in range(2)]
    ost = [P.sb([128, 512], F32, "ost") for _ in range(3)]
    io = 0
    iw = 0
    for g in range(4):
        x_ = xg[g % 2]
        for k4 in range(4):
            P.dma("sp", lambda e: e.dma_start(out=x_[:, k4 * 4:(k4 + 1) * 4, :],
                  in_=mixT_hbm[k4 * 512:(k4 + 1) * 512, g * 512:(g + 1) * 512].rearrange("(k p) t -> p k t", p=128)),
                  reads=[C.mixT_b], writes=[x_])
        for cc in range(4):
            w_ = wb[iw % 2]; iw += 1
            for k4 in range(4):
                P.dma("pool", lambda e: e.dma_start(out=w_[:, k4 * 4:(k4 + 1) * 4, :],
                      in_=w_out[k4 * 512:(k4 + 1) * 512, cc * 512:(cc + 1) * 512].rearrange("(k p) c -> p k c", p=128)), writes=[w_])
            for q in range(4):
                it = g * 4 + q
                py = psf(C, (0, 1, 2, 3))
                for kc in range(16):
                    P.op("pe", lambda e: e.matmul(py[:], lhsT=x_[:, kc, q * 128:(q + 1) * 128], rhs=w_[:, kc, :], start=(kc == 0), stop=(kc == 15)),
                         reads=[x_, w_], writes=[py])
                o = ost[io % 3]; io += 1
                if io % 2:
                    P.op("act", lambda e: e.copy(out=o[:], in_=py[:]), reads=[py], writes=[o])
                else:
                    P.op("dve", lambda e: e.tensor_copy(out=o[:], in_=py[:]), reads=[py], writes=[o])
                P.dma("sp", lambda e: e.dma_start(out=mix_hbm[it * 128:(it + 1) * 128, cc * 512:(cc + 1) * 512], in_=o[:]), reads=[o])
    P.barrier()


RW_NAMES = ("w0", "w2", "a0", "a2", "g2", "k_k", "k_a", "r_k", "ln_w", "ln_b")
IN_SPECS = [
    ("x", [S, D]),
    ("ev_w_in", [2, D, 6432]), ("ev_w_out", [2, 2048, D]), ("rw_mu", [2, 3360]), ("rw_w0", [2, 1024]), ("rw_w2", [2, 64, 1024]),
    ("rw_a0", [2, 1024]), ("rw_a2", [2, 64, 1024]), ("rw_g2", [2, 160, 1024]), ("rw_k_k", [2, 1024]), ("rw_k_a", [2, 1024]),
    ("rw_r_k", [2, 1024]), ("rw_ln_w", [2, 1024]), ("rw_ln_b", [2, 1024]), ("rw_v0", [1, 1024]), ("rw_v1", [1, 1024, 32]),
    ("rw_v2", [1, 32, 1024]), ("od_w_in", [2, D, 1088]), ("od_q_norm", [2, 512]), ("od_kv_norm", [2, 512]), ("od_w_uq", [2, 512, 3072]),
    ("od_w_ukv", [2, 512, 4096]), ("od_w_out", [2, 2048, D]), ("ln_mix_g", [4, D]), ("ln_mix_b", [4, D]), ("ln_ffn_g", [4, D]),
    ("ln_ffn_b", [4, D]), ("moe_w_r", [4, D, NE]), ("moe_b_r", [4, NE]), ("moe_w1", [4, NE, D, 2 * DEXP]), ("moe_b1", [4, NE, 2 * DEXP]),
    ("moe_w2", [4, NE, DEXP, D]), ("moe_b2", [4, NE, D]),
]


def build_program(n_layers=DEPTH):
    nc = bass.Bass("TRN2", target_bir_lowering=False)
    I = {nm: nc.dram_tensor(nm, list(shp), F32, kind="ExternalInput").ap() for nm, shp in IN_SPECS}
    out = nc.dram_tensor("out", [S, D], F32, kind="ExternalOutput").ap()
    hA = nc.dram_tensor("hA", [S, D], F32, kind="Internal").ap()
    hB = nc.dram_tensor("hB", [S, D], F32, kind="Internal").ap()
    mix = nc.dram_tensor("mix", [S, D], F32, kind="Internal").ap()
    mixT = nc.dram_tensor("mixT", [2048, S], BF16, kind="Internal").ap()
    pp = nc.dram_tensor("pp", [RW_IN, S], F32, kind="Internal").ap()
    vf = nc.dram_tensor("vfirst", [1024, S], F32, kind="Internal").ap()
    P = Prog(nc)
    C = Ctx()
    alloc_persist(nc, P, C)
    C.pp_b, C.sc_b, C.vf_b, C.mixT_b = Buf(), Buf(), Buf(), Buf()
    moe = new_moe(nc, C, "0")
    h_cur = I["x"]
    for layer in range(n_layers):
        j = layer // 2
        last = (layer == n_layers - 1)
        if layer == 0:
            stage_load_hT(C, h_cur)
        if layer % 2 == 0:
            stage_moba(C, w_in=I["ev_w_in"][j], mixT_hbm=mixT)
            stage_rwkv_proj(C, w_in=I["ev_w_in"][j], mu_row=I["rw_mu"][j:j + 1, :], pp_hbm=pp)
            prm = {}
            for nm in RW_NAMES:
                a = I["rw_" + nm]
                prm[nm] = a[j] if len(a.shape) == 3 else a[j:j + 1, :]
            if j > 0:
                prm["v0"] = I["rw_v0"][j - 1:j, :]
                prm["v1"] = I["rw_v1"][j - 1]
                prm["v2"] = I["rw_v2"][j - 1]
            stage_rwkv_main(C, pp_hbm=pp, prm=prm, vfirst_hbm=vf, first=(j == 0), mixT_hbm=mixT)
            stage_outproj_hbm(C, mixT_hbm=mixT, w_out=I["ev_w_out"][j], mix_hbm=mix)
        else:
            stage_mla(C, w_in=I["od_w_in"][j], q_norm=I["od_q_norm"][j:j + 1, :], kv_norm=I["od_kv_norm"][j:j + 1, :],
                      w_uq=I["od_w_uq"][j], w_ukv=I["od_w_ukv"][j], w_out=I["od_w_out"][j], mix_hbm=mix)
        m1 = dict(moe)
        m1.update(route=True, w_r=I["moe_w_r"][layer], b_r=I["moe_b_r"][layer:layer + 1, :])
        stage_ln(C, h_in=h_cur, add_mode="hbm", mix_hbm=mix, h_out=hA, g_row=I["ln_mix_g"][layer:layer + 1, :],
                 b_row=I["ln_mix_b"][layer:layer + 1, :], moe=m1)
        stage_moe(C, moe=moe, w1=I["moe_w1"][layer], b1=I["moe_b1"][layer], w2=I["moe_w2"][layer], b2=I["moe_b2"][layer])
        m2 = dict(moe)
        m2.update(route=False)
        stage_ln(C, h_in=hA, add_mode="moe", moe=m2, h_out=hB, out_final=(out if last else None),
                 g_row=I["ln_ffn_g"][layer:layer + 1, :], b_row=I["ln_ffn_b"][layer:layer + 1, :], hT=(None if last else C.hT))
        h_cur = hB
    P.emit()
    return nc, P


_CACHE = {}


def kernel(**inputs):
    n = 8
    if "nc" not in _CACHE:
        _CACHE["nc"] = build_program()
    nc, P = _CACHE["nc"]
    shared = {}
    for nm, shp in IN_SPECS:
        if nm == "x":
            continue
        shared[nm] = np.ascontiguousarray(np.asarray(inputs[nm], dtype=np.float32).reshape(shp))
    x = np.asarray(inputs["x"], dtype=np.float32)
    in_maps = []
    for b in range(n):
        m = dict(shared)
        m["x"] = np.ascontiguousarray(x[b])
        in_maps.append(m)
    res = run_bass_kernel_spmd(nc, in_maps, core_ids=list(range(n)))
    return np.stack([np.asarray(r["out"]) for r in res.results], axis=0).astype(np.float32)
```
